# Optimizing a Trainium2 kernel written in Bass

```python
import math
import jax, jax.numpy as jnp
from jax import lax
import numpy as np

D_MODEL = 2048
BATCH = 16
SEQ = 2048
DEPTH = 4

F32 = jnp.float32
N_MEM = 256
Q_BLOCK = 128
DIFF_HEADS = D_MODEL // 256
DIFF_DQK = 32
DIFF_DV = 2 * DIFF_DQK
SWA_HEADS = D_MODEL // 128
SWA_KV_HEADS = 2
SWA_DH = 64
SWA_WINDOW = 128
GLA_HEADS = D_MODEL // 512
GLA_DK = 64
GLA_DV = 128
GLA_RANK = 16
GLA_TAU = 16.0
GLA_CHUNK = 64
D_DIFF = DIFF_HEADS * DIFF_DV
D_SWA = SWA_HEADS * SWA_DH
D_GLA = GLA_HEADS * GLA_DV
D_MIX = D_DIFF + D_SWA + D_GLA
IN_WIDTHS = (
    DIFF_HEADS * 2 * DIFF_DQK,
    DIFF_HEADS * 2 * DIFF_DQK,
    D_DIFF,
    D_SWA,
    SWA_KV_HEADS * SWA_DH,
    SWA_KV_HEADS * SWA_DH,
    GLA_HEADS * GLA_DK,
    GLA_HEADS * GLA_DK,
    D_GLA,
    GLA_RANK,
    D_GLA,
)
D_IN = sum(IN_WIDTHS)
SPLIT_POINTS = [int(v) for v in np.cumsum(IN_WIDTHS)[:-1]]
XA_HEADS = 4
XA_DH = 128
D_XA = XA_HEADS * XA_DH
D_FF = int(math.ceil(8 * D_MODEL / 3 / 256)) * 256
CONV_W = 3
EPS = 1e-6

kernel_name = "hybrid_diff_swa_gla_trunk"


def rmsnorm(x, g):
    xf = x.astype(F32)
    y = xf * lax.rsqrt(jnp.mean(xf * xf, axis=-1, keepdims=True) + EPS)
    return (y * g.astype(F32)).astype(x.dtype)


def alibi_slopes(n):
    return 2.0 ** (-8.0 * jnp.arange(1, n + 1, dtype=F32) / n)


def diff_attention(q, k, v, lam, subln_g, lambda_init):
    B, S, H, _, dq = q.shape
    dv = v.shape[-1]
    nb = S // Q_BLOCK
    scale = dq ** -0.5
    slopes = alibi_slopes(H)
    lamf = lam.astype(F32)
    lam_full = jnp.exp(jnp.sum(lamf[0] * lamf[1])) - jnp.exp(jnp.sum(lamf[2] * lamf[3])) + lambda_init
    pos_k = jnp.arange(S)
    qb = q.reshape(B, nb, Q_BLOCK, H, 2, dq).transpose(1, 0, 2, 3, 4, 5)

    def block(args):
        qi, n = args
        pos_q = n * Q_BLOCK + jnp.arange(Q_BLOCK)
        dist = pos_q[:, None] - pos_k[None, :]
        s = jnp.einsum('bqhcd,bkhcd->bhcqk', qi, k).astype(F32) * scale
        s = s - slopes[None, :, None, None, None] * dist.astype(F32)
        s = jnp.where(dist >= 0, s, -jnp.inf)
        p = jax.nn.softmax(s, axis=-1)
        a = p[:, :, 0] - lam_full * p[:, :, 1]
        return jnp.einsum('bhqk,bkhd->bqhd', a.astype(v.dtype), v)

    o = lax.map(block, (qb, jnp.arange(nb)))
    o = o.transpose(1, 0, 2, 3, 4).reshape(B, S, H, dv)
    o = rmsnorm(o, subln_g) * (1.0 - lambda_init)
    return o.reshape(B, S, H * dv)


def swa_attention(q, k, v, sinks):
    B, S, Hq, dh = q.shape
    G = k.shape[2]
    R = Hq // G
    W = SWA_WINDOW
    nb = S // W
    slopes = alibi_slopes(Hq).reshape(G, R)
    qb = q.reshape(B, nb, W, G, R, dh)

    def with_prev(t):
        tb = t.reshape(B, nb, W, G, dh)
        prev = jnp.concatenate([jnp.zeros_like(tb[:, :1]), tb[:, :-1]], axis=1)
        return jnp.concatenate([prev, tb], axis=2)

    kw, vw = with_prev(k), with_prev(v)
    s = jnp.einsum('bnqgrd,bnkgd->bngrqk', qb, kw).astype(F32) * (dh ** -0.5)
    i = jnp.arange(W)[:, None]
    j = jnp.arange(2 * W)[None, :]
    dist = i + W - j
    blk = jnp.arange(nb)[:, None, None]
    valid = (dist >= 0) & (dist < W) & (blk * W - W + j[None] >= 0)
    s = s - slopes[:, :, None, None] * dist.astype(F32)
    s = jnp.where(valid[None, :, None, None], s, -jnp.inf)
    sink = jnp.broadcast_to(sinks.astype(F32).reshape(G, R)[None, None, :, :, None, None], s.shape[:-1] + (1,))
    p = jax.nn.softmax(jnp.concatenate([s, sink], axis=-1), axis=-1)[..., :-1]
    o = jnp.einsum('bngrqk,bnkgd->bnqgrd', p.astype(v.dtype), vw)
    return o.reshape(B, S, Hq * dh)


def gla_attention(q, k, v, log_a):
    B, S, H, dk = q.shape
    dv = v.shape[-1]
    C = GLA_CHUNK
    nc = S // C

    def chunk(t):
        return t.astype(F32).reshape(B, nc, C, H, t.shape[-1])

    qc = chunk(q) * (dk ** -0.5)
    kc, vc, gc = chunk(k), chunk(v), chunk(log_a)
    b = jnp.cumsum(gc, axis=2)
    b_last = b[:, :, -1:]
    q_dec = qc * jnp.exp(b)
    k_inv = kc * jnp.exp(-b)
    k_end = kc * jnp.exp(b_last - b)
    causal = jnp.tril(jnp.ones((C, C), dtype=bool))
    attn = jnp.where(causal, jnp.einsum('bnthd,bnshd->bnhts', q_dec, k_inv), 0.0)
    o_intra = jnp.einsum('bnhts,bnshv->bnthv', attn, vc)
    kv = jnp.einsum('bnshd,bnshv->bnhdv', k_end, vc)
    decay = jnp.exp(b_last[:, :, 0])

    def step(state, inp):
        kv_n, dec_n = inp
        return state * dec_n[..., None] + kv_n, state

    init = jnp.zeros((B, H, dk, dv), F32)
    _, states = lax.scan(step, init, (kv.transpose(1, 0, 2, 3, 4), decay.transpose(1, 0, 2, 3)))
    states = states.transpose(1, 0, 2, 3, 4)
    o_inter = jnp.einsum('bnthd,bnhdv->bnthv', q_dec, states)
    return (o_intra + o_inter).reshape(B, S, H, dv)


def cross_attention(xn, memn, wq, wkv, wo):
    B, S, _ = xn.shape
    M = memn.shape[1]
    q = (xn @ wq).reshape(B, S, XA_HEADS, XA_DH)
    k, v = jnp.split(memn @ wkv, 2, axis=-1)
    k = k.reshape(B, M, XA_HEADS, XA_DH)
    v = v.reshape(B, M, XA_HEADS, XA_DH)
    s = jnp.einsum('bshd,bmhd->bhsm', q, k).astype(F32) * (XA_DH ** -0.5)
    p = jax.nn.softmax(s, axis=-1)
    o = jnp.einsum('bhsm,bmhd->bshd', p.astype(v.dtype), v).reshape(B, S, D_XA)
    return o @ wo


def conv_glu(xn, w_up, conv_w, conv_b, w_down):
    h = xn @ w_up
    hp = jnp.pad(h, ((0, 0), (CONV_W - 1, 0), (0, 0)))
    h = hp[:, :-2] * conv_w[0] + hp[:, 1:-1] * conv_w[1] + hp[:, 2:] * conv_w[2] + conv_b
    gate, up = jnp.split(h, 2, axis=-1)
    return (jax.nn.silu(gate) * up) @ w_down


def setup_inputs(seed: int = 0) -> dict:
    key = jax.random.key(seed)
    ks = jax.random.split(key, 24)
    L, D = DEPTH, D_MODEL

    def nrm(k, shape, scale):
        return jax.random.normal(k, shape, F32) * scale

    def gain(k, shape):
        return 1.0 + 0.02 * jax.random.normal(k, shape, F32)

    return {
        "x": nrm(ks[0], (BATCH, SEQ, D), 1.0),
        "mem": nrm(ks[1], (BATCH, N_MEM, D), 1.0),
        "norm_mix_g": gain(ks[2], (L, D)),
        "w_in": nrm(ks[3], (L, D, D_IN), D ** -0.5),
        "diff_lambda": nrm(ks[4], (L, 4, DIFF_DQK), 0.1),
        "diff_subln_g": gain(ks[5], (L, DIFF_DV)),
        "swa_sinks": nrm(ks[6], (L, SWA_HEADS), 1.0),
        "gla_gate_w2": nrm(ks[7], (L, GLA_RANK, GLA_HEADS * GLA_DK), GLA_RANK ** -0.5),
        "gla_gate_b": nrm(ks[8], (L, GLA_HEADS * GLA_DK), 0.02),
        "gla_norm_g": gain(ks[9], (L, GLA_DV)),
        "w_out": nrm(ks[10], (L, D_MIX, D), D_MIX ** -0.5),
        "norm_xa_g": gain(ks[11], (L, D)),
        "norm_mem_g": gain(ks[12], (L, D)),
        "xa_wq": nrm(ks[13], (L, D, D_XA), D ** -0.5),
        "xa_wkv": nrm(ks[14], (L, D, 2 * D_XA), D ** -0.5),
        "xa_wo": nrm(ks[15], (L, D_XA, D), D_XA ** -0.5),
        "norm_ffn_g": gain(ks[16], (L, D)),
        "ffn_w_up": nrm(ks[17], (L, D, 2 * D_FF), D ** -0.5),
        "ffn_conv_w": nrm(ks[18], (L, CONV_W, 2 * D_FF), CONV_W ** -0.5),
        "ffn_conv_b": nrm(ks[19], (L, 2 * D_FF), 0.02),
        "ffn_w_down": nrm(ks[20], (L, D_FF, D), D_FF ** -0.5),
        "final_norm_g": gain(ks[21], (D,)),
    }


def reference(x, mem, norm_mix_g, w_in, diff_lambda, diff_subln_g, swa_sinks, gla_gate_w2, gla_gate_b, gla_norm_g, w_out, norm_xa_g, norm_mem_g, xa_wq, xa_wkv, xa_wo, norm_ffn_g, ffn_w_up, ffn_conv_w, ffn_conv_b, ffn_w_down, final_norm_g):
    B, S, _ = x.shape
    h = x
    for l in range(DEPTH):
        xn = rmsnorm(h, norm_mix_g[l])
        (d_q, d_k, d_v, s_q, s_k, s_v, g_q, g_k, g_v, g_lr, g_og) = jnp.split(xn @ w_in[l], SPLIT_POINTS, axis=-1)
        lambda_init = 0.8 - 0.6 * math.exp(-0.3 * l)
        y_diff = diff_attention(
            d_q.reshape(B, S, DIFF_HEADS, 2, DIFF_DQK),
            d_k.reshape(B, S, DIFF_HEADS, 2, DIFF_DQK),
            d_v.reshape(B, S, DIFF_HEADS, DIFF_DV),
            diff_lambda[l], diff_subln_g[l], lambda_init)
        y_swa = swa_attention(
            s_q.reshape(B, S, SWA_HEADS, SWA_DH),
            s_k.reshape(B, S, SWA_KV_HEADS, SWA_DH),
            s_v.reshape(B, S, SWA_KV_HEADS, SWA_DH),
            swa_sinks[l])
        z = (g_lr @ gla_gate_w2[l] + gla_gate_b[l]).astype(F32)
        log_a = (jax.nn.log_sigmoid(z) / GLA_TAU).reshape(B, S, GLA_HEADS, GLA_DK)
        o_gla = gla_attention(
            g_q.reshape(B, S, GLA_HEADS, GLA_DK),
            g_k.reshape(B, S, GLA_HEADS, GLA_DK),
            g_v.reshape(B, S, GLA_HEADS, GLA_DV),
            log_a)
        y_gla = (rmsnorm(o_gla, gla_norm_g[l]).reshape(B, S, D_GLA) * jax.nn.silu(g_og.astype(F32))).astype(x.dtype)
        mix = jnp.concatenate([y_diff.astype(x.dtype), y_swa.astype(x.dtype), y_gla], axis=-1)
        h = h + mix @ w_out[l]
        h = h + cross_attention(rmsnorm(h, norm_xa_g[l]), rmsnorm(mem, norm_mem_g[l]), xa_wq[l], xa_wkv[l], xa_wo[l])
        h = h + conv_glu(rmsnorm(h, norm_ffn_g[l]), ffn_w_up[l], ffn_conv_w[l], ffn_conv_b[l], ffn_w_down[l])
    return rmsnorm(h, final_norm_g)
```

```python
import math
import os
import numpy as np
import concourse.bass as bass
import concourse.mybir as mybir
from concourse.bass_utils import run_bass_kernel_spmd

F32 = mybir.dt.float32
BF16 = mybir.dt.bfloat16
AF = mybir.ActivationFunctionType
ALU = mybir.AluOpType

D = 2048
S = 2048
NSEQ = 2
DEPTH = 4
NMEM = 256
D_IN = 4368
D_FF = 5632
EPS = 1e-6
NEG = -30000.0
OFF = dict(dq=0, dk=512, dv=1024, sq=1536, sk=2560, sv=2688, gq=2816, gk=3072, gv=3328, glr=3840, gog=3856)
ARENA0 = 20480
ARENA_END = 229000


def _dsz(dt):
    return 4 if dt == F32 else 2


class Reg:
    __slots__ = ("w", "r", "key", "name", "psum")

    def __init__(self, name):
        self.w = None
        self.r = {}
        self.key = None
        self.name = name
        self.psum = False


class V:
    __slots__ = ("ap", "regs")

    def __init__(self, ap, regs):
        self.ap = ap
        self.regs = regs


class Buf:
    def __init__(self, t, nreg=1, name="anon"):
        self.t = t
        self.regs = [Reg("%s.%d" % (name, i)) for i in range(nreg)]

    def __getitem__(self, idx):
        return V(self.t[idx], [self.regs[0]])

    def rg(self, reg, idx):
        return V(self.t[idx], [self.regs[reg]])


class Rot:
    def __init__(self, items):
        self.items = items
        self.i = 0

    def next(self):
        x = self.items[self.i % len(self.items)]
        self.i += 1
        return x


class KB:
    def __init__(self, nc):
        self.nc = nc
        self.eng = dict(pe=nc.tensor, act=nc.scalar, dve=nc.vector, pool=nc.gpsimd, sp=nc.sync)
        self.sems = {}
        self.cnt = {}
        self.seen = {}
        self.ekey = {}
        self.gen = {}
        for e in self.eng:
            self.gen[e] = 0
            self.ekey[e] = e + "#0"
            self.sems[self.ekey[e]] = nc.alloc_semaphore("s_" + e + "_0")
            self.cnt[self.ekey[e]] = 0
            self.seen[e] = {}
        self.ndma = 0
        self.uid = 0
        self.keyof = {}
        self.dtot = {}
        self.nmm = 0
        self.marks = []

    def sb(self, name, shape, dt, off, nreg=1):
        self.uid += 1
        t = self.nc.alloc_sbuf_tensor_at("%s_%d" % (name, self.uid), list(shape), dt, offset=off)
        return Buf(t, nreg, name)

    def _need(self, E, reads, writes, skipkey=None):
        need = {}
        for v in reads:
            for g in v.regs:
                if g.w is not None:
                    k, c = g.w
                    if need.get(k, 0) < c:
                        need[k] = c
                if g.psum:
                    for k, c in g.r.items():
                        if not k.startswith(E + "#") and need.get(k, 0) < c:
                            need[k] = c
        for v in writes:
            for g in v.regs:
                if g.w is not None:
                    k, c = g.w
                    if need.get(k, 0) < c:
                        need[k] = c
                for k, c in g.r.items():
                    if need.get(k, 0) < c:
                        need[k] = c
        sn = self.seen[E]
        for k, c in need.items():
            if k == skipkey:
                continue
            if E == "pe" and k.startswith("pe#"):
                continue
            if sn.get(k, 0) < c:
                self.eng[E].wait_ge(self.sems[k], c)
                sn[k] = c

    def op(self, E, fn, reads, writes, sig=True):
        self._need(E, reads, writes)
        inst = fn(self.eng[E])
        ek = self.ekey[E]
        if sig:
            self.cnt[ek] += 1
            c = self.cnt[ek]
            inst.then_inc(self.sems[ek], 1)
        else:
            c = self.cnt[ek] + 1
        for v in reads:
            for g in v.regs:
                g.r[ek] = c
        for v in writes:
            for g in v.regs:
                g.w = (ek, c)
                g.r = {}

    def dma(self, Q, out, in_):
        g = out.regs[0]
        if g.key is None:
            if g.name not in self.keyof:
                self.ndma += 1
                k = "d%d" % self.ndma
                self.keyof[g.name] = k
                self.sems[k] = self.nc.alloc_semaphore(k)
                self.dtot[k] = 0
            g.key = self.keyof[g.name]
        self._need(Q, [in_], [out], skipkey=g.key)
        inst = self.eng[Q].dma_start(out=out.ap, in_=in_.ap)
        self.dtot[g.key] += 16
        c = self.dtot[g.key]
        inst.then_inc(self.sems[g.key], 16)
        for rg in in_.regs:
            rg.r[g.key] = c
        g.w = (g.key, c)
        g.r = {}

    def barrier(self):
        tot = {}
        for e in self.eng:
            ek = self.ekey[e]
            if self.cnt[ek] > 0:
                tot[ek] = self.cnt[ek]
        for E in self.eng:
            sn = self.seen[E]
            for k, c in tot.items():
                if k == self.ekey[E]:
                    continue
                if sn.get(k, 0) < c:
                    self.eng[E].wait_ge(self.sems[k], c)
                    sn[k] = c
            for k, c in self.dtot.items():
                if sn.get(k, 0) < c:
                    self.eng[E].wait_ge(self.sems[k], c)
                    sn[k] = c
        for e in self.eng:
            ek = self.ekey[e]
            if self.cnt[ek] > 12000:
                self.gen[e] += 1
                nk = "%s#%d" % (e, self.gen[e])
                self.ekey[e] = nk
                self.sems[nk] = self.nc.alloc_semaphore("s_%s_%d" % (e, self.gen[e]))
                self.cnt[nk] = 0

    def maybe_barrier(self, limit=24000):
        if any(self.cnt[self.ekey[e]] > limit for e in self.eng):
            self.barrier()

    def mark(self, name):
        self.marks.append((name, self.nmm))

    def mm(self, out, lhsT, rhs, start=True, stop=True, sig=True):
        self.nmm += 1
        self.op("pe", lambda e: e.matmul(out.ap, lhsT.ap, rhs.ap, start=start, stop=stop), [lhsT, rhs], [out], sig=sig)

    def act(self, out, in_, func, scale=1.0, bias=None, E="act"):
        rd = [in_]
        kw = {}
        if bias is not None:
            rd.append(bias)
            kw["bias"] = bias.ap
        if isinstance(scale, V):
            rd.append(scale)
            kw["scale"] = scale.ap
        else:
            kw["scale"] = float(scale)
        self.op("act", lambda e: e.activation(out=out.ap, in_=in_.ap, func=func, **kw), rd, [out])

    def tt(self, out, in0, in1, op, E="dve"):
        self.op(E, lambda e: e.tensor_tensor(out=out.ap, in0=in0.ap, in1=in1.ap, op=op), [in0, in1], [out])

    def ts(self, out, in0, s1, s2, op0, op1=None, E="dve"):
        rd = [in0]
        a1 = s1
        a2 = s2
        if isinstance(s1, V):
            rd.append(s1)
            a1 = s1.ap
        if isinstance(s2, V):
            rd.append(s2)
            a2 = s2.ap
        if op1 is None:
            self.op(E, lambda e: e.tensor_scalar(out=out.ap, in0=in0.ap, scalar1=a1, scalar2=None, op0=op0), rd, [out])
        else:
            self.op(E, lambda e: e.tensor_scalar(out=out.ap, in0=in0.ap, scalar1=a1, scalar2=a2, op0=op0, op1=op1), rd, [out])

    def stt(self, out, in0, sc, in1, op0, op1, E="dve"):
        rd = [in0, in1]
        a = sc
        if isinstance(sc, V):
            rd.append(sc)
            a = sc.ap
        self.op(E, lambda e: e.scalar_tensor_tensor(out=out.ap, in0=in0.ap, scalar=a, in1=in1.ap, op0=op0, op1=op1), rd, [out])

    def recip(self, out, in_):
        self.act(out, in_, AF.Ln)
        self.act(out, out, AF.Exp, scale=-1.0)

    def copy(self, out, in_, E="dve"):
        if E == "act":
            self.act(out, in_, AF.Copy)
        else:
            self.op(E, lambda e: e.tensor_copy(out=out.ap, in_=in_.ap), [in_], [out])

    def memset(self, out, val, E="dve"):
        self.op(E, lambda e: e.memset(out.ap, val), [], [out])


def pipeline(units, A, B, depth=2):
    n = len(units)
    for k in range(min(depth, n)):
        A(units[k])
    for k in range(n):
        if k + depth < n:
            A(units[k + depth])
        B(units[k])


class Arena:
    def __init__(self, kb, start, end):
        self.kb = kb
        self.start = start
        self.end = end
        self.cur = start
        self.hist = []

    def reset(self, to=None):
        self.cur = self.start if to is None else to

    def alloc(self, name, shape, dt, nreg=1):
        n = 1
        for s in shape[1:]:
            n *= s
        nbytes = (n * _dsz(dt) + 63) // 64 * 64
        off = self.cur
        assert off + nbytes <= self.end, "SBUF arena overflow for %s: %d + %d > %d" % (name, off, nbytes, self.end)
        self.cur += nbytes
        b = self.kb.sb(name, shape, dt, off, nreg)
        keep = []
        per = nbytes // nreg
        rng = [(off + i * per, off + (i + 1) * per if i < nreg - 1 else off + nbytes, b.regs[i]) for i in range(nreg)]
        for (s0, e0, r0) in self.hist:
            covered = (off <= s0 and e0 <= off + nbytes)
            for (lo, hi, g) in rng:
                if s0 < hi and lo < e0:
                    if r0.w is not None:
                        k, c = r0.w
                        if g.r.get(k, 0) < c:
                            g.r[k] = c
                    for k, c in r0.r.items():
                        if g.r.get(k, 0) < c:
                            g.r[k] = c
            if not covered:
                keep.append((s0, e0, r0))
        keep.extend(rng)
        self.hist = keep
        return b


def build_program(nseq=NSEQ, nlayer=DEPTH, dbg=None):
    nc = bass.Bass("TRN2", target_bir_lowering=False)
    kb = KB(nc)

    def din(name, shape):
        return nc.dram_tensor(name, list(shape), F32, kind="ExternalInput").ap()

    xT = din("xT", [NSEQ, D, S])
    memT = din("memT", [NSEQ, D, NMEM])
    w_in = din("w_in", [DEPTH, D, D_IN])
    w_out = din("w_out", [DEPTH, D, D])
    xa_wq = din("xa_wq", [DEPTH, D, 512])
    xa_wkv = din("xa_wkv", [DEPTH, D, 1024])
    xa_wo = din("xa_wo", [DEPTH, 512, D])
    w_up = din("ffn_w_up", [DEPTH, D, 2 * D_FF])
    w_down = din("ffn_w_down", [DEPTH, D_FF, D])
    gains = din("gains", [DEPTH, 128, 64])
    gfin = din("gfin", [128, 16])
    colpack = din("colpack", [DEPTH, 128, 146])
    convp = din("convp", [DEPTH, 128, 4, 88])
    w2aug = din("w2aug", [DEPTH, 32, 256])
    cD = din("cD", [128, 2048])
    cmask = din("cmask", [128, 256])
    cU = din("cU", [128, 256])
    augk = din("augk", [8, 4, S])
    augq = din("augq", [8, 4, S])
    yT = nc.dram_tensor("yT", [NSEQ, D, S], F32, kind="ExternalOutput").ap()
    hres = nc.dram_tensor("hres", [NSEQ, D, S], F32, kind="Internal").ap()
    hresB = Buf(None, NSEQ * 2, "hres")
    yTB = Buf(None, NSEQ * 2, "yT")
    inB = Buf(None, 1, "inputs")

    def dv(ap):
        return V(ap, [inB.regs[0]])

    PS = [Buf(nc.alloc_psum_tensor("ps%d" % i, [128, 512], F32), 1, "ps%d" % i) for i in range(8)]
    for b in PS:
        b.regs[0].psum = True
    psS = Rot(PS[0:3])
    psO = Rot(PS[3:5])
    psD = Rot(PS[5:7])
    psX = Rot(PS[7:8])
    psS5 = Rot(PS[0:3] + PS[5:7])
    psO3 = Rot(PS[3:5] + PS[7:8])

    PA = Arena(kb, ARENA0, ARENA0 + 16 * 1024)
    cDt = PA.alloc("cD", [128, 2048], F32)
    cmaskt = PA.alloc("cmask", [128, 256], F32)
    cUt = PA.alloc("cU", [128, 256], F32)
    cUb = PA.alloc("cUb", [128, 128], BF16)
    ones = PA.alloc("ones", [128, 128], BF16)
    gt = PA.alloc("gains", [128, 64], F32)
    gs = PA.alloc("gains_s", [128, 64], F32)
    gfint = PA.alloc("gfin", [128, 16], F32)
    colt = PA.alloc("colpack", [128, 146], F32)
    colx = PA.alloc("colx", [128, 24], F32)
    convt = PA.alloc("convp", [128, 4, 88], F32)
    w2t = PA.alloc("w2aug", [32, 256], BF16)
    carry = PA.alloc("carry", [128, 88, 2], F32)
    epsc = PA.alloc("epsc", [128, 4], F32)
    WA = Arena(kb, PA.end, ARENA_END)

    kb.dma("sp", cDt[:, :], dv(cD[:, :]))
    kb.dma("sp", cmaskt[:, :], dv(cmask[:, :]))
    kb.dma("sp", cUt[:, :], dv(cU[:, :]))
    kb.dma("sp", gfint[:, :], dv(gfin[:, :]))
    kb.memset(ones[:, :], 1.0)
    kb.copy(cUb[:, :], cUt[:, 0:128])
    kb.memset(epsc[:, 0:1], 1.0)
    kb.memset(epsc[:, 1:2], D * EPS)
    kb.memset(epsc[:, 2:3], 64.0 * EPS)
    kb.memset(epsc[:, 3:4], 128.0 * EPS)
    kb.ts(gfint[:, :], gfint[:, :], math.sqrt(D), None, ALU.mult)

    slopes8 = [2.0 ** (-8.0 * (i + 1) / 8) for i in range(8)]
    slopes16 = [2.0 ** (-8.0 * (i + 1) / 16) for i in range(16)]

    def hsrc(l, s):
        return (xT, inB, 0) if l == 0 else (hres, hresB, None)

    def wview(W, l, c0, nc_):
        return dv(W[l, :, c0:c0 + nc_].rearrange("(kc p) n -> p kc n", p=128))

    def rsqrt_to(out, in_, col, M):
        kb.act(out, in_, AF.Ln, bias=epsc[0:M, col:col + 1])
        kb.act(out, out, AF.Exp, scale=-0.5)

    def rms_stats(src_fn, nk, N, sqrot, rs_out, nfeat):
        ps = psX.next()
        for kc in range(nk):
            sq = sqrot.next()
            kb.act(sq[:, 0:N], src_fn(kc), AF.Square)
            kb.mm(ps[:, 0:N], ones[:, :], sq[:, 0:N], start=(kc == 0), stop=(kc == nk - 1))
        rsqrt_to(rs_out, ps[:, 0:N], 1, 128)

    for s in range(nseq):
        for l in range(nlayer):
            lam_init = 0.8 - 0.6 * math.exp(-0.3 * l)
            kb.dma("sp", gt[:, :], dv(gains[l, :, :]))
            kb.dma("sp", colt[:, :], dv(colpack[l, :, :]))
            kb.dma("sp", convt[:, :, :], dv(convp[l, :, :, :]))
            kb.dma("pool", w2t[:, :], dv(w2aug[l, :, :]))
            kb.ts(gs[:, :], gt[:, :], math.sqrt(D), None, ALU.mult)
            kb.ts(colx[:, 0:1], colt[:, 0:1], (1.0 - lam_init) * 8.0, None, ALU.mult)
            kb.ts(colx[:, 1:2], colt[:, 1:2], math.sqrt(128.0), None, ALU.mult)
            kb.act(colx[:, 4:20], colt[:, 2:18], AF.Exp)
            WA.reset()
            lsc = WA.alloc("lsc", [128, 64], F32)
            kb.tt(lsc[:, 0:32], colt[:, 18:50], colt[:, 50:82], ALU.mult)
            kb.tt(lsc[:, 32:64], colt[:, 82:114], colt[:, 114:146], ALU.mult)
            kb.op("dve", lambda e: e.reduce_sum(out=colx[:, 20:21].ap, in_=lsc[:, 0:32].ap, axis=mybir.AxisListType.X), [lsc[:, :]], [colx[:, :]])
            kb.op("dve", lambda e: e.reduce_sum(out=colx[:, 21:22].ap, in_=lsc[:, 32:64].ap, axis=mybir.AxisListType.X), [lsc[:, :]], [colx[:, :]])
            kb.act(colx[:, 22:24], colx[:, 20:22], AF.Exp)
            kb.stt(colx[:, 2:3], colx[:, 23:24], -lam_init, colx[:, 22:23], ALU.add, ALU.subtract)

            src, srcB, srcreg = hsrc(l, s)

            def sreg(half):
                return 0 if srcreg == 0 else s * 2 + half

            WA.reset()
            xn = WA.alloc("xn", [128, 16, S], BF16)
            mixT = WA.alloc("mixT", [128, 16, S], BF16)
            mark = WA.cur
            kb.mark("norm")
            hsts = Rot([WA.alloc("hst%d" % i, [128, 16, 256], F32) for i in range(2)])
            sq2 = [WA.alloc("sq%d" % i, [128, 256], BF16) for i in range(3)]
            rss = Rot([WA.alloc("rs%d" % i, [128, 256], F32) for i in range(2)])
            sqrot = Rot(sq2)
            for t8 in range(S // 256):
                tsl = slice(t8 * 256, (t8 + 1) * 256)
                hst = hsts.next()
                rs = rss.next()
                kb.dma("sp", hst[:, :, :], V(src[s, :, tsl].rearrange("(kc p) t -> p kc t", p=128), [srcB.regs[sreg(t8 // 4)]]))
                rms_stats(lambda kc: hst[:, kc, :], 16, 256, sqrot, rs[:, :], D)
                for kc in range(16):
                    kb.stt(xn[:, kc, tsl], hst[:, kc, :], gs[:, kc:kc + 1], rs[:, :], ALU.mult, ALU.mult)
            WA.reset(mark)
            wb = [WA.alloc("wbm%d" % i, [128, 16, 256], BF16) for i in range(2)]
            wrot = Rot(wb)
            mark2 = WA.cur

            def wload(W, c0, ncols):
                w = wrot.next()
                kb.dma("pool", w[:, :, 0:ncols], wview(W, l, c0, ncols))
                return w

            def proj_fm(w, c0, M, dst_fn, func=AF.Copy, scale=1.0):
                for tl in range(4):
                    ps = psS.next()
                    for kc in range(16):
                        kb.mm(ps[0:M, :], w[:, kc, c0:c0 + M], xn[:, kc, tl * 512:(tl + 1) * 512], start=(kc == 0), stop=(kc == 15), sig=(kc == 15))
                    d = dst_fn(tl)
                    if isinstance(d, list):
                        for (dd, r0, r1) in d:
                            kb.act(dd, ps[r0:r1, :], func, scale=scale)
                    else:
                        kb.act(d, ps[0:M, :], func, scale=scale)

            def proj_tm(w, c0, N, dst_fn, split=None):
                for blk in range(16):
                    ps = psS.next()
                    for kc in range(16):
                        kb.mm(ps[:, 0:N], xn[:, kc, blk * 128:(blk + 1) * 128], w[:, kc, c0:c0 + N], start=(kc == 0), stop=(kc == 15), sig=(kc == 15))
                    if split is None:
                        kb.act(dst_fn(blk), ps[:, 0:N], AF.Copy)
                    else:
                        src_v = V(ps.t[:, 0:N].rearrange("p (h d) -> p h d", h=split[0]), ps.regs)
                        kb.act(dst_fn(blk), src_v, AF.Copy)

            kb.mark("diff")
            sc = 32.0 ** -0.5
            qTh = WA.alloc("qTh", [128, S], BF16)
            kTh = WA.alloc("kTh", [128, S], BF16)
            vdf = WA.alloc("vdf", [128, 16, 4, 128], BF16)
            kb.memset(vdf[:, :, :, 64:128], 1.0)
            Tb = Rot([WA.alloc("T%d" % i, [128, 512], F32) for i in range(2)])
            Pb = Rot([WA.alloc("P%d" % i, [128, 512], BF16) for i in range(4)])
            Rc = WA.alloc("Rc", [64, 512], F32)
            Oc = [WA.alloc("Oc%d" % i, [64, 512], F32) for i in range(2)]
            sqdr = Rot([WA.alloc("sqd%d" % i, [64, 512], BF16) for i in range(1)])
            Oo = Rot([WA.alloc("Oo%d" % i, [64, 512], F32) for i in range(1)])
            rsd = WA.alloc("rsd", [64, 512], F32)
            for hg in range(2):
                wv = wload(w_in, OFF["dv"] + hg * 256, 256)
                proj_tm(wv, 0, 256, lambda blk: vdf[:, blk, :, 0:64], split=(4, 64))
                wq = wload(w_in, OFF["dq"] + hg * 256, 256)
                wk = wload(w_in, OFF["dk"] + hg * 256, 256)
                for hh in range(4):
                    h = hg * 4 + hh
                    if hh == 0:
                        pass
                    proj_fm(wq, hh * 64, 64, lambda tl: [(qTh[0:32, tl * 512:(tl + 1) * 512], 0, 32),
                                                         (qTh[64:96, tl * 512:(tl + 1) * 512], 32, 64)], func=AF.Identity, scale=sc)
                    proj_fm(wk, hh * 64, 64, lambda tl: [(kTh[0:32, tl * 512:(tl + 1) * 512], 0, 32),
                                                         (kTh[64:96, tl * 512:(tl + 1) * 512], 32, 64)])
                    for b0 in (32, 96):
                        kb.dma("pool", qTh[b0:b0 + 4, :], dv(augq[h, :, :]))
                        kb.dma("pool", kTh[b0:b0 + 4, :], dv(augk[h, :, :]))
                    slope = slopes8[h]
                    units = []
                    for i in range(4):
                        for c in range(2):
                            nj = 4 * i + 4
                            for j in range(nj):
                                units.append((i, c, j, nj))
                    Pof = {}
                    acc_of = {}

                    def stA(u, h=h, hh=hh, slope=slope):
                        i, c, j, nj = u
                        qa = max(512 * i, 128 * j)
                        N = 512 * (i + 1) - qa
                        Sp = psS5.next()
                        kb.mm(Sp[:, 0:N], kTh[64 * c:64 * c + 36, 128 * j:128 * j + 128], qTh[64 * c:64 * c + 36, qa:qa + N])
                        P = Pb.next()
                        if qa == 128 * j:
                            T = Tb.next()
                            kb.tt(T[:, 0:128], Sp[:, 0:128], cmaskt[:, 0:128], ALU.add)
                            kb.act(P[:, 0:128], T[:, 0:128], AF.Exp)
                            if N > 128:
                                kb.act(P[:, 128:N], Sp[:, 128:N], AF.Exp)
                        else:
                            kb.act(P[:, 0:N], Sp[:, 0:N], AF.Exp)
                        Pof[u] = P

                    def stB(u, h=h, hh=hh):
                        i, c, j, nj = u
                        qa = max(512 * i, 128 * j)
                        N = 512 * (i + 1) - qa
                        off = qa - 512 * i
                        if j == 0:
                            acc_of[(i, c)] = psO.next()
                        O = acc_of[(i, c)]
                        P = Pof.pop(u)
                        kb.mm(O[:, off:off + N], vdf[:, j, hh, :], P[:, 0:N], start=(j == 0), stop=(j == nj - 1))
                        if j == nj - 1:
                            kb.recip(Rc[:, :], O[64:128, :])
                            kb.tt(Oc[c][:, :], O[0:64, :], Rc[:, :], ALU.mult)
                            if c == 1:
                                oo = Oo.next()
                                sqq = sqdr.next()
                                kb.stt(oo[:, :], Oc[1][:, :], colx[0:64, 2:3], Oc[0][:, :], ALU.mult, ALU.add)
                                kb.act(sqq[:, :], oo[:, :], AF.Square)

                                def tail(i=i, oo=oo, sqq=sqq, h=h):
                                    pn = psX.next()
                                    kb.mm(pn[0:64, :], ones[0:64, 0:64], sqq[:, :])
                                    rsqrt_to(rsd[:, :], pn[0:64, :], 2, 64)
                                    po = (h % 2) * 64
                                    kb.stt(mixT[po:po + 64, h // 2, i * 512:(i + 1) * 512], oo[:, :], colx[0:64, 0:1], rsd[:, :], ALU.mult, ALU.mult)

                                deferred.append([3, tail])
                        for dt_ in deferred:
                            dt_[0] -= 1
                        while deferred and deferred[0][0] <= 0:
                            deferred.pop(0)[1]()

                    deferred = []
                    pipeline(units, stA, stB, 3)
                    while deferred:
                        deferred.pop(0)[1]()
            if dbg == "diff":
                return finish_dbg(nc, kb, mixT, yT, yTB, s)
            WA.reset(mark2)
            kb.mark("swa")
            sc = 64.0 ** -0.5
            qTs = WA.alloc("qTs", [128, 4, S], BF16)
            kTs = WA.alloc("kTs", [128, S], BF16)
            vsw = WA.alloc("vsw", [128, 16, 2, 128], BF16)
            kb.memset(vsw[:, :, :, 64:128], 1.0)
            Tb = Rot([WA.alloc("T%d" % i, [128, 512], F32) for i in range(2)])
            Pb = Rot([WA.alloc("P%d" % i, [128, 512], BF16) for i in range(4)])
            Rs = WA.alloc("Rs", [64, 512], F32)
            bdiag = WA.alloc("bdiag", [128, 512], F32)
            bprev = WA.alloc("bprev", [128, 512], F32)
            wkv = wload(w_in, OFF["sk"], 256)
            proj_fm(wkv, 0, 128, lambda tl: kTs[:, tl * 512:(tl + 1) * 512])
            proj_tm(wkv, 128, 128, lambda blk: vsw[:, blk, :, 0:64], split=(2, 64))
            for rh in range(2):
                for g in range(2):
                    w = wload(w_in, OFF["sq"] + (8 * g + 4 * rh) * 64, 256)
                    for pr in range(2):
                        rrA = 2 * pr
                        proj_fm(w, pr * 128, 128, lambda tl, rrA=rrA, g=g: [
                            (qTs[g * 64:(g + 1) * 64, rrA, tl * 512:(tl + 1) * 512], 0, 64),
                            (qTs[g * 64:(g + 1) * 64, rrA + 1, tl * 512:(tl + 1) * 512], 64, 128)])
                for g in range(2):
                    for rr in range(4):
                        slope = slopes16[8 * g + 4 * rh + rr]
                        seg = slice(rr * 128, (rr + 1) * 128)
                        kb.stt(bdiag[:, seg], cDt[:, 0:128], -slope / sc, cmaskt[:, 0:128], ALU.mult, ALU.add)
                        kb.stt(bprev[:, seg], cDt[:, 128:256], -slope / sc, cmaskt[:, 128:256], ALU.mult, ALU.add)
                    units = []
                    for n in range(16):
                        ms = [n - 1, n] if n > 0 else [n]
                        for idx, m in enumerate(ms):
                            units.append((n, m, idx, len(ms)))
                    Pof = {}
                    acc_of = {}

                    def stA(u, g=g):
                        n, m, idx, nm = u
                        Sp = psS5.next()
                        kb.mm(Sp[:, :], kTs[g * 64:(g + 1) * 64, m * 128:(m + 1) * 128], qTs[g * 64:(g + 1) * 64, :, n * 128:(n + 1) * 128])
                        T = Tb.next()
                        kb.tt(T[:, :], Sp[:, :], (bdiag if m == n else bprev)[:, :], ALU.add)
                        P = Pb.next()
                        kb.act(P[:, :], T[:, :], AF.Exp, scale=sc)
                        Pof[u] = P

                    def stB(u, g=g, rh=rh):
                        n, m, idx, nm = u
                        if idx == 0:
                            acc_of[n] = psO3.next()
                        O = acc_of[n]
                        P = Pof.pop(u)
                        kb.mm(O[:, :], vsw[:, m, g, :], P[:, :], start=(idx == 0), stop=(idx == nm - 1))
                        if idx == nm - 1:
                            for rr in range(4):
                                hd = 8 * g + 4 * rh + rr
                                kb.ts(Rs[:, rr * 128:(rr + 1) * 128], O[64:128, rr * 128:(rr + 1) * 128], colx[64:128, 4 + hd:5 + hd], None, ALU.add)
                            kb.recip(Rs[:, :], Rs[:, :])
                            for rr in range(4):
                                hd = 8 * g + 4 * rh + rr
                                po = (hd % 2) * 64
                                kb.tt(mixT[po:po + 64, 4 + hd // 2, n * 128:(n + 1) * 128], O[0:64, rr * 128:(rr + 1) * 128], Rs[:, rr * 128:(rr + 1) * 128], ALU.mult)

                    pipeline(units, stA, stB, 3)
            if dbg == "swa":
                return finish_dbg(nc, kb, mixT, yT, yTB, s)
            WA.reset(mark2)
            kb.mark("gla")
            glr = WA.alloc("glr", [32, S], BF16)
            qTg = WA.alloc("qTg", [64, S], BF16)
            kTg = WA.alloc("kTg", [64, S], BF16)
            ktk = WA.alloc("ktk", [128, 16, 64], BF16)
            vtk = WA.alloc("vtk", [128, 16, 128], BF16)
            sgo = WA.alloc("sgo", [128, S], BF16)
            lap = Rot([WA.alloc("lap%d" % i, [128, 64], F32) for i in range(2)])
            e1b = Rot([WA.alloc("e1%d" % i, [128, 64], F32) for i in range(2)])
            eeb = Rot([WA.alloc("ee%d" % i, [128, 64], F32) for i in range(2)])
            kend = Rot([WA.alloc("kend%d" % i, [128, 64], BF16) for i in range(2)])
            ebb = Rot([WA.alloc("eb%d" % i, [64, 128], F32) for i in range(2)])
            ebi = Rot([WA.alloc("ebi%d" % i, [64, 128], F32) for i in range(2)])
            qdec = Rot([WA.alloc("qdec%d" % i, [64, 128], BF16) for i in range(2)])
            kinv = Rot([WA.alloc("kinv%d" % i, [64, 128], BF16) for i in range(2)])
            attn = Rot([WA.alloc("attn%d" % i, [128, 128], BF16) for i in range(2)])
            Sf = Rot([WA.alloc("Sf%d" % i, [64, 128], F32) for i in range(3)])
            Sb = Rot([WA.alloc("Sb%d" % i, [64, 128], BF16) for i in range(3)])
            sqg = Rot([WA.alloc("sqg%d" % i, [128, 128], BF16) for i in range(2)])
            rsg = Rot([WA.alloc("rsg%d" % i, [128, 128], F32) for i in range(2)])
            t1g = Rot([WA.alloc("t1g%d" % i, [128, 128], F32) for i in range(2)])
            kb.memset(glr[:, :], 1.0)
            wl = wload(w_in, OFF["glr"], 16)
            proj_fm(wl, 0, 16, lambda tl: glr[0:16, tl * 512:(tl + 1) * 512])
            for hh in range(4):
                w = wload(w_in, OFF["gq"] + hh * 64, 64)
                proj_fm(w, 0, 64, lambda tl: qTg[:, tl * 512:(tl + 1) * 512])
                w = wload(w_in, OFF["gk"] + hh * 64, 64)
                proj_fm(w, 0, 64, lambda tl: kTg[:, tl * 512:(tl + 1) * 512])
                proj_tm(w, 0, 64, lambda blk: ktk[:, blk, :])
                w = wload(w_in, OFF["gv"] + hh * 128, 128)
                proj_tm(w, 0, 128, lambda blk: vtk[:, blk, :])
                w = wload(w_in, OFF["gog"] + hh * 128, 128)
                proj_fm(w, 0, 128, lambda tl: sgo[:, tl * 512:(tl + 1) * 512], func=AF.Silu)
                st = {}
                st["Sfc"] = Sf.next()
                st["Sbc"] = Sb.next()
                kb.memset(st["Sfc"][:, :], 0.0)
                kb.memset(st["Sbc"][:, :], 0.0)
                blk = {}

                def gA(nb, hh=hh):
                    bs = slice(nb * 128, (nb + 1) * 128)
                    z = psS.next()
                    kb.mm(z[:, 0:64], glr[0:17, bs], w2t[0:17, hh * 64:(hh + 1) * 64])
                    e1 = e1b.next()
                    kb.act(e1[:, :], z[:, 0:64], AF.Exp, scale=-1.0)
                    lp = lap.next()
                    kb.act(lp[:, :], e1[:, :], AF.Ln, bias=epsc[:, 0:1])
                    et = psS.next()
                    kb.mm(et[:, 0:64], cUt[:, 128:256], lp[:, :])
                    bT = psS.next()
                    kb.mm(bT[0:64, 0:128], lp[:, :], cUt[:, 0:128])
                    ee = eeb.next()
                    kb.act(ee[:, :], et[:, 0:64], AF.Exp, scale=-1.0 / 16.0)
                    ke = kend.next()
                    kb.tt(ke[:, :], ktk[:, nb, :], ee[:, :], ALU.mult)
                    eb = ebb.next()
                    kb.act(eb[:, :], bT[0:64, 0:128], AF.Exp, scale=-1.0 / 16.0)
                    ei = ebi.next()
                    kb.act(ei[:, :], bT[0:64, 0:128], AF.Exp, scale=1.0 / 16.0)
                    qd = qdec.next()
                    kb.stt(qd[:, :], qTg[:, bs], 0.125, eb[:, :], ALU.mult, ALU.mult)
                    ki = kinv.next()
                    kb.tt(ki[:, :], kTg[:, bs], ei[:, :], ALU.mult)
                    at = psS.next()
                    kb.mm(at[:, 0:128], ki[:, :], qd[:, :])
                    an = attn.next()
                    kb.tt(an[:, :], at[:, 0:128], cUt[:, 0:128], ALU.mult)
                    blk[nb] = (ke, eb, qd, an)

                def gB(nb, hh=hh):
                    bs = slice(nb * 128, (nb + 1) * 128)
                    ke, eb, qd, an = blk.pop(nb)
                    Sfc = st["Sfc"]
                    Sbc = st["Sbc"]
                    o = psO.next()
                    kb.mm(o[:, 0:128], vtk[:, nb, :], an[:, :], start=True, stop=False)
                    kb.mm(o[:, 0:64], Sbc[:, :], qd[:, 0:64], start=False, stop=False)
                    kv = psD.next()
                    kb.mm(kv[0:64, 0:128], ke[0:64, :], vtk[0:64, nb, :])
                    Sfn = Sf.next()
                    kb.stt(Sfn[:, :], Sfc[:, :], eb[:, 63:64], kv[0:64, 0:128], ALU.mult, ALU.add)
                    Sbn = Sb.next()
                    kb.copy(Sbn[:, :], Sfn[:, :], E="act")
                    kb.mm(o[:, 64:128], Sbn[:, :], qd[:, 64:128], start=False, stop=True)
                    kv = psD.next()
                    kb.mm(kv[0:64, 0:128], ke[64:128, :], vtk[64:128, nb, :])
                    Sfc = Sf.next()
                    kb.stt(Sfc[:, :], Sfn[:, :], eb[:, 127:128], kv[0:64, 0:128], ALU.mult, ALU.add)
                    Sbc = Sb.next()
                    kb.copy(Sbc[:, :], Sfc[:, :], E="act")
                    st["Sfc"] = Sfc
                    st["Sbc"] = Sbc
                    sq = sqg.next()
                    kb.act(sq[:, :], o[:, 0:128], AF.Square)
                    pn = psX.next()
                    kb.mm(pn[:, 0:128], ones[:, :], sq[:, :])
                    rg_ = rsg.next()
                    rsqrt_to(rg_[:, :], pn[:, 0:128], 3, 128)
                    t1 = t1g.next()
                    kb.stt(t1[:, :], o[:, 0:128], colx[:, 1:2], rg_[:, :], ALU.mult, ALU.mult)
                    kb.tt(mixT[:, 12 + hh, bs], t1[:, :], sgo[:, bs], ALU.mult)

                pipeline(list(range(16)), gA, gB, 1)
            if dbg == "gla":
                return finish_dbg(nc, kb, mixT, yT, yTB, s)
            WA.reset(mark2)
            kb.mark("wout")
            hold = Rot([WA.alloc("hold%d" % i, [128, 512], F32) for i in range(4)])
            for cg in range(8):
                w = wload(w_out, cg * 256, 256)
                for dc in range(2):
                    dch = cg * 2 + dc
                    for tl in range(4):
                        tsl = slice(tl * 512, (tl + 1) * 512)
                        ho = hold.next()
                        kb.dma("sp", ho[:, :], V(src[s, dch * 128:(dch + 1) * 128, tsl], [srcB.regs[sreg(tl // 2)]]))
                        ps = psS.next()
                        for kc in range(16):
                            kb.mm(ps[:, :], w[:, kc, dc * 128:(dc + 1) * 128], mixT[:, kc, tsl], start=(kc == 0), stop=(kc == 15), sig=(kc == 15))
                        kb.tt(ho[:, :], ho[:, :], ps[:, :], ALU.add)
                        kb.dma("sp", V(hres[s, dch * 128:(dch + 1) * 128, tsl], [hresB.regs[s * 2 + tl // 2]]), ho[:, :])
            if dbg == "mix":
                return finish_dbg2(nc, kb, hres, hresB, yT, yTB, s)

            WA.reset()
            kTx = WA.alloc("kTx", [128, 4, 256], BF16)
            vx = WA.alloc("vx", [128, 2, 512], BF16)
            markKV = WA.cur
            kb.mark("xkv")
            mst = WA.alloc("mst", [128, 16, 128], F32)
            memn = WA.alloc("memn", [128, 16, 256], BF16)
            wbk = Rot([WA.alloc("wbk%d" % i, [128, 16, 256], BF16) for i in range(2)])
            sqk = Rot([WA.alloc("sqk%d" % i, [128, 128], BF16) for i in range(2)])
            rsk = WA.alloc("rsk", [128, 128], F32)
            for mb in range(2):
                kb.dma("sp", mst[:, :, :], dv(memT[s, :, mb * 128:(mb + 1) * 128].rearrange("(kc p) t -> p kc t", p=128)))
                rms_stats(lambda kc: mst[:, kc, :], 16, 128, sqk, rsk[:, :], D)
                for kc in range(16):
                    kb.stt(memn[:, kc, mb * 128:(mb + 1) * 128], mst[:, kc, :], gs[:, 32 + kc:33 + kc], rsk[:, :], ALU.mult, ALU.mult)
            for cgk in range(2):
                w = wbk.next()
                kb.dma("pool", w[:, :, :], wview(xa_wkv, l, cgk * 256, 256))
                for hx2 in range(2):
                    hx = cgk * 2 + hx2
                    ps = psS.next()
                    for kc in range(16):
                        kb.mm(ps[:, 0:256], w[:, kc, hx2 * 128:(hx2 + 1) * 128], memn[:, kc, :], start=(kc == 0), stop=(kc == 15), sig=(kc == 15))
                    kb.act(kTx[:, hx, :], ps[:, 0:256], AF.Copy)
            for cgv in range(2):
                w = wbk.next()
                kb.dma("pool", w[:, :, :], wview(xa_wkv, l, 512 + cgv * 256, 256))
                for mb in range(2):
                    ps = psS.next()
                    for kc in range(16):
                        kb.mm(ps[:, 0:256], memn[:, kc, mb * 128:(mb + 1) * 128], w[:, kc, :], start=(kc == 0), stop=(kc == 15), sig=(kc == 15))
                    kb.act(vx[:, mb, cgv * 256:(cgv + 1) * 256], ps[:, 0:256], AF.Copy)
            for hf in range(2):
                WA.reset(markKV)
                T0 = hf * 1024
                acc = WA.alloc("acc", [128, 16, 1024], F32, nreg=16)

                def A(kc, sl):
                    return acc.rg(kc, (slice(None), kc, sl))

                xn2 = WA.alloc("xn2", [128, 16, 1024], BF16)
                wb = [WA.alloc("wb%d" % i, [128, 16, 256], BF16) for i in range(3)]
                wrot = Rot(wb)
                sq2 = [WA.alloc("sq%d" % i, [128, 512], BF16) for i in range(2)]
                sqrot = Rot(sq2)
                rs5 = WA.alloc("rs5", [128, 512], F32)
                markT = WA.cur
                for kc in range(16):
                    kb.dma("sp", A(kc, slice(None)), V(hres[s, kc * 128:(kc + 1) * 128, T0:T0 + 1024], [hresB.regs[s * 2 + hf]]))

                def norm_to_xn2(gcol0):
                    for tl in range(2):
                        tsl = slice(tl * 512, (tl + 1) * 512)
                        rms_stats(lambda kc: A(kc, tsl), 16, 512, sqrot, rs5[:, :], D)
                        for kc in range(16):
                            kb.stt(xn2[:, kc, tsl], A(kc, tsl), gs[:, gcol0 + kc:gcol0 + kc + 1], rs5[:, :], ALU.mult, ALU.mult)

                kb.mark("xa%d" % hf)
                qTx = WA.alloc("qTx", [128, 4, 1024], BF16)
                oxa = WA.alloc("oxa", [128, 4, 1024], BF16)
                wo = WA.alloc("wo", [128, 4, 2048], BF16)
                Rx = WA.alloc("Rx", [128, 512], F32)
                Pb = Rot([WA.alloc("P%d" % i, [128, 512], BF16) for i in range(3)])
                norm_to_xn2(16)
                for cgq in range(2):
                    w = wrot.next()
                    kb.dma("pool", w[:, :, :], wview(xa_wq, l, cgq * 256, 256))
                    for hx2 in range(2):
                        hx = cgq * 2 + hx2
                        for tl in range(2):
                            ps = psS.next()
                            for kc in range(16):
                                kb.mm(ps[:, :], w[:, kc, hx2 * 128:(hx2 + 1) * 128], xn2[:, kc, tl * 512:(tl + 1) * 512], start=(kc == 0), stop=(kc == 15), sig=(kc == 15))
                            kb.act(qTx[:, hx, tl * 512:(tl + 1) * 512], ps[:, :], AF.Copy)
                kb.dma("pool", wo[:, :, :], dv(xa_wo[l, :, :].rearrange("(kc p) n -> p kc n", p=128)))
                scx = 128.0 ** -0.5
                units = []
                for hx in range(4):
                    for tl in range(2):
                        for mb in range(2):
                            units.append((hx, tl, mb))
                Pof = {}
                acc_of = {}

                def xA(u):
                    hx, tl, mb = u
                    tsl = slice(tl * 512, (tl + 1) * 512)
                    Sp = psS.next()
                    kb.mm(Sp[:, :], kTx[:, hx, mb * 128:(mb + 1) * 128], qTx[:, hx, tsl])
                    P = Pb.next()
                    kb.act(P[:, :], Sp[:, :], AF.Exp, scale=scx)
                    Pof[u] = P

                def xB(u):
                    hx, tl, mb = u
                    tsl = slice(tl * 512, (tl + 1) * 512)
                    if mb == 0:
                        acc_of[(hx, tl)] = (psO.next(), psD.next())
                    O, Dn = acc_of[(hx, tl)]
                    P = Pof.pop(u)
                    kb.mm(O[:, :], vx[:, mb, hx * 128:(hx + 1) * 128], P[:, :], start=(mb == 0), stop=(mb == 1))
                    kb.mm(Dn[:, :], ones[:, :], P[:, :], start=(mb == 0), stop=(mb == 1))
                    if mb == 1:
                        kb.recip(Rx[:, :], Dn[:, :])
                        kb.tt(oxa[:, hx, tsl], O[:, :], Rx[:, :], ALU.mult)

                pipeline(units, xA, xB, 2)
                for dch in range(16):
                    for tl in range(2):
                        tsl = slice(tl * 512, (tl + 1) * 512)
                        ps = psS.next()
                        for kc in range(4):
                            kb.mm(ps[:, :], wo[:, kc, dch * 128:(dch + 1) * 128], oxa[:, kc, tsl], start=(kc == 0), stop=(kc == 3), sig=(kc == 3))
                        kb.tt(A(dch, tsl), A(dch, tsl), ps[:, :], ALU.add)
                if dbg == "xa":
                    return finish_dbg3(nc, kb, acc, yT, yTB, s, hf)
                WA.reset(markT)
                kb.mark("ffn%d" % hf)
                norm_to_xn2(48)
                G = 4
                wdn = Rot([WA.alloc("wd%d" % i, [128, 2048], BF16) for i in range(7)])
                actb = Rot([WA.alloc("act%d" % i, [128, 1024], BF16) for i in range(7)])
                ag = Rot([WA.alloc("ag%d" % i, [128, 512], F32) for i in range(2)])
                au = Rot([WA.alloc("au%d" % i, [128, 512], F32) for i in range(2)])
                sgb = Rot([WA.alloc("sg%d" % i, [128, 512], F32) for i in range(2)])
                psG = Rot(PS[0:4])
                psDn = Rot(PS[4:7])
                npair = D_FF // 128
                def ffn_down(grp):
                    for dch in range(16):
                        for tl in range(2):
                            tsl = slice(tl * 512, (tl + 1) * 512)
                            ps = psDn.next()
                            for ki, (wd, ab) in enumerate(grp):
                                kb.mm(ps[:, :], wd[:, dch * 128:(dch + 1) * 128], ab[:, tsl], start=(ki == 0), stop=(ki == len(grp) - 1), sig=(ki == len(grp) - 1))
                            kb.tt(A(dch, tsl), A(dch, tsl), ps[:, :], ALU.add)

                pending = None
                grp = []
                for p in range(npair):
                    wu = wrot.next()
                    kb.dma("pool", wu[:, :, 0:128], wview(w_up, l, p * 128, 128))
                    kb.dma("pool", wu[:, :, 128:256], wview(w_up, l, D_FF + p * 128, 128))
                    wd = wdn.next()
                    kb.dma("pool", wd[:, :], dv(w_down[l, p * 128:(p + 1) * 128, :]))
                    ab = actb.next()
                    grp.append((wd, ab))
                    for tl in range(2):
                        tsl = slice(tl * 512, (tl + 1) * 512)
                        pg = psG.next()
                        pu = psG.next()
                        for kc in range(16):
                            kb.mm(pg[:, :], wu[:, kc, 0:128], xn2[:, kc, tsl], start=(kc == 0), stop=(kc == 15), sig=(kc == 15))
                        for kc in range(16):
                            kb.mm(pu[:, :], wu[:, kc, 128:256], xn2[:, kc, tsl], start=(kc == 0), stop=(kc == 15), sig=(kc == 15))
                        first = (hf == 0 and tl == 0)
                        res = []
                        for (pp, ch, abuf) in ((pg, p, ag), (pu, npair + p, au)):
                            a = abuf.next()
                            kb.act(a[:, :], pp[:, :], AF.Identity, scale=convt[:, 2, ch:ch + 1], bias=convt[:, 3, ch:ch + 1])
                            kb.stt(a[:, 1:512], pp[:, 0:511], convt[:, 1, ch:ch + 1], a[:, 1:512], ALU.mult, ALU.add)
                            kb.stt(a[:, 2:512], pp[:, 0:510], convt[:, 0, ch:ch + 1], a[:, 2:512], ALU.mult, ALU.add)
                            if not first:
                                kb.stt(a[:, 0:1], carry[:, ch, 1:2], convt[:, 1, ch:ch + 1], a[:, 0:1], ALU.mult, ALU.add)
                                kb.stt(a[:, 0:2], carry[:, ch, 0:2], convt[:, 0, ch:ch + 1], a[:, 0:2], ALU.mult, ALU.add)
                            kb.copy(carry[:, ch, :], pp[:, 510:512], E="act")
                            res.append(a)
                        sg = sgb.next()
                        kb.act(sg[:, :], res[0][:, :], AF.Silu)
                        kb.tt(ab[:, tsl], sg[:, :], res[1][:, :], ALU.mult)
                    if pending is not None:
                        ffn_down(pending)
                        pending = None
                    if len(grp) == G or p == npair - 1:
                        pending = grp
                        grp = []
                if pending is not None:
                    ffn_down(pending)
                if l == nlayer - 1:
                    for tl in range(2):
                        tsl = slice(tl * 512, (tl + 1) * 512)
                        rms_stats(lambda kc: A(kc, tsl), 16, 512, sqrot, rs5[:, :], D)
                        for kc in range(16):
                            kb.stt(A(kc, tsl), A(kc, tsl), gfint[:, kc:kc + 1], rs5[:, :], ALU.mult, ALU.mult)
                    for kc in range(16):
                        kb.dma("sp", V(yT[s, kc * 128:(kc + 1) * 128, T0:T0 + 1024], [yTB.regs[s * 2 + hf]]), A(kc, slice(None)))
                else:
                    for kc in range(16):
                        kb.dma("sp", V(hres[s, kc * 128:(kc + 1) * 128, T0:T0 + 1024], [hresB.regs[s * 2 + hf]]), A(kc, slice(None)))
            kb.maybe_barrier()
    kb.mark("end")
    finish(nc, kb)
    global LAST_KB
    LAST_KB = kb
    return nc


def _collect_dtot(kb, bufs):
    pass


def finish(nc, kb):
    kb.barrier()


def finish_dbg(nc, kb, mixT, yT, yTB, s):
    for kc in range(16):
        kb.dma("pool", V(yT[s, kc * 128:(kc + 1) * 128, :], [yTB.regs[0]]), mixT[:, kc, :])
    kb.barrier()
    return nc


def finish_dbg2(nc, kb, hres, hresB, yT, yTB, s):
    kb.dma("sp", V(yT[s, :, :], [yTB.regs[0]]), V(hres[s, :, :], [hresB.regs[2 * s], hresB.regs[2 * s + 1]]))
    kb.barrier()
    return nc


def finish_dbg3(nc, kb, acc, yT, yTB, s, hf):
    for kc in range(16):
        kb.dma("sp", V(yT[s, kc * 128:(kc + 1) * 128, hf * 1024:(hf + 1) * 1024], [yTB.regs[0]]), V(acc.t[:, kc, :], acc.regs))
    kb.barrier()
    return nc


def host_consts():
    p = np.arange(128, dtype=np.float32)[:, None]
    u = np.arange(2048, dtype=np.float32)[None, :]
    cD = (u - p).astype(np.float32)
    c = np.arange(128)[None, :]
    pp = np.arange(128)[:, None]
    diag = np.where(c >= pp, 0.0, NEG).astype(np.float32)
    prev = np.where(c < pp, 0.0, NEG).astype(np.float32)
    cmask = np.concatenate([diag, prev], axis=1)
    same = (pp // 64) == (c // 64)
    uincl = (same & (pp <= c)).astype(np.float32)
    uafter = (same & (pp > c)).astype(np.float32)
    cU = np.concatenate([uincl, uafter], axis=1)
    return cD, cmask, cU


def prep_shared(inp):
    f = np.float32
    L = DEPTH

    def col16(g):
        return np.ascontiguousarray(g.reshape(L, 16, 128).transpose(0, 2, 1))

    gains = np.concatenate([col16(inp["norm_mix_g"]), col16(inp["norm_xa_g"]), col16(inp["norm_mem_g"]), col16(inp["norm_ffn_g"])], axis=2).astype(f)
    gfin = np.ascontiguousarray(inp["final_norm_g"].reshape(16, 128).T).astype(f)
    colpack = np.zeros((L, 128, 146), f)
    colpack[:, :, 0] = np.concatenate([inp["diff_subln_g"], inp["diff_subln_g"]], axis=1)
    colpack[:, :, 1] = inp["gla_norm_g"]
    colpack[:, :, 2:18] = inp["swa_sinks"][:, None, :]
    colpack[:, :, 18:146] = inp["diff_lambda"].reshape(L, 1, 128)
    cw = inp["ffn_conv_w"].reshape(L, 3, 88, 128)
    cb = inp["ffn_conv_b"].reshape(L, 1, 88, 128)
    convp = np.ascontiguousarray(np.concatenate([cw, cb], axis=1).transpose(0, 3, 1, 2)).astype(f)
    w2aug = np.zeros((L, 32, 256), f)
    w2aug[:, 0:16, :] = inp["gla_gate_w2"]
    w2aug[:, 16, :] = inp["gla_gate_b"]
    cD, cmask, cU = host_consts()
    tpos = np.arange(S)
    ka = (tpos // 128).astype(f)
    kbb = (tpos % 128).astype(f)
    augk = np.zeros((8, 4, S), f)
    augq = np.zeros((8, 4, S), f)
    for hh_ in range(8):
        sl = 2.0 ** (-(hh_ + 1))
        augk[hh_, 0] = ka
        augk[hh_, 1] = kbb
        augk[hh_, 2] = sl
        augk[hh_, 3] = sl
        augq[hh_, 0] = 128.0 * sl
        augq[hh_, 1] = sl
        augq[hh_, 2] = -128.0 * ka
        augq[hh_, 3] = -kbb
    sh = dict(w_in=inp["w_in"], w_out=inp["w_out"], xa_wq=inp["xa_wq"], xa_wkv=inp["xa_wkv"], xa_wo=inp["xa_wo"],
              ffn_w_up=inp["ffn_w_up"], ffn_w_down=inp["ffn_w_down"], gains=gains, gfin=gfin, colpack=colpack,
              convp=convp, w2aug=w2aug, cD=cD, cmask=cmask, cU=cU, augk=augk, augq=augq)
    return {k: np.ascontiguousarray(v, dtype=f) for k, v in sh.items()}


def kernel(**inputs):
    inp = {k: np.asarray(v) for k, v in inputs.items()}
    ncores = 8
    shared = prep_shared(inp)
    x = inp["x"]
    mem = inp["mem"]
    in_maps = []
    for c in range(ncores):
        m = dict(shared)
        m["xT"] = np.ascontiguousarray(x[c * NSEQ:(c + 1) * NSEQ].transpose(0, 2, 1), dtype=np.float32)
        m["memT"] = np.ascontiguousarray(mem[c * NSEQ:(c + 1) * NSEQ].transpose(0, 2, 1), dtype=np.float32)
        in_maps.append(m)
    nc = build_program()
    res = run_bass_kernel_spmd(nc, in_maps, core_ids=list(range(ncores)))
    out = np.empty((16, S, D), np.float32)
    for c in range(ncores):
        yT = res.results[c]["yT"]
        out[c * NSEQ:(c + 1) * NSEQ] = yT.transpose(0, 2, 1)
    return out
```

```python
import math
import os
import numpy as np
import concourse.bass as bass
import concourse.mybir as mybir
from concourse.bass_utils import run_bass_kernel_spmd

F32 = mybir.dt.float32
BF16 = mybir.dt.bfloat16
AF = mybir.ActivationFunctionType
ALU = mybir.AluOpType

D = 2048
S = 2048
NSEQ = 2
DEPTH = 4
NMEM = 256
D_IN = 4368
D_FF = 5632
EPS = 1e-6
NEG = -30000.0
OFF = dict(dq=0, dk=512, dv=1024, sq=1536, sk=2560, sv=2688, gq=2816, gk=3072, gv=3328, glr=3840, gog=3856)
ARENA0 = 20480
ARENA_END = 229000


def _dsz(dt):
    return 4 if dt == F32 else 2


class Reg:
    __slots__ = ("w", "r", "key", "name", "psum")

    def __init__(self, name):
        self.w = None
        self.r = {}
        self.key = None
        self.name = name
        self.psum = False


class V:
    __slots__ = ("ap", "regs")

    def __init__(self, ap, regs):
        self.ap = ap
        self.regs = regs


class Buf:
    def __init__(self, t, nreg=1, name="anon"):
        self.t = t
        self.regs = [Reg("%s.%d" % (name, i)) for i in range(nreg)]

    def __getitem__(self, idx):
        return V(self.t[idx], [self.regs[0]])

    def rg(self, reg, idx):
        return V(self.t[idx], [self.regs[reg]])


class Rot:
    def __init__(self, items):
        self.items = items
        self.i = 0

    def next(self):
        x = self.items[self.i % len(self.items)]
        self.i += 1
        return x


class KB:
    def __init__(self, nc):
        self.nc = nc
        self.eng = dict(pe=nc.tensor, act=nc.scalar, dve=nc.vector, pool=nc.gpsimd, sp=nc.sync)
        self.sems = {}
        self.cnt = {}
        self.seen = {}
        self.ekey = {}
        self.gen = {}
        for e in self.eng:
            self.gen[e] = 0
            self.ekey[e] = e + "#0"
            self.sems[self.ekey[e]] = nc.alloc_semaphore("s_" + e + "_0")
            self.cnt[self.ekey[e]] = 0
            self.seen[e] = {}
        self.ndma = 0
        self.uid = 0
        self.keyof = {}
        self.dtot = {}
        self.nmm = 0
        self.marks = []

    def sb(self, name, shape, dt, off, nreg=1):
        self.uid += 1
        t = self.nc.alloc_sbuf_tensor_at("%s_%d" % (name, self.uid), list(shape), dt, offset=off)
        return Buf(t, nreg, name)

    def _need(self, E, reads, writes, skipkey=None):
        need = {}
        for v in reads:
            for g in v.regs:
                if g.w is not None:
                    k, c = g.w
                    if need.get(k, 0) < c:
                        need[k] = c
                if g.psum:
                    for k, c in g.r.items():
                        if not k.startswith(E + "#") and need.get(k, 0) < c:
                            need[k] = c
        for v in writes:
            for g in v.regs:
                if g.w is not None:
                    k, c = g.w
                    if need.get(k, 0) < c:
                        need[k] = c
                for k, c in g.r.items():
                    if need.get(k, 0) < c:
                        need[k] = c
        sn = self.seen[E]
        for k, c in need.items():
            if k == skipkey:
                continue
            if E == "pe" and k.startswith("pe#"):
                continue
            if sn.get(k, 0) < c:
                self.eng[E].wait_ge(self.sems[k], c)
                sn[k] = c

    def op(self, E, fn, reads, writes, sig=True):
        self._need(E, reads, writes)
        inst = fn(self.eng[E])
        ek = self.ekey[E]
        if sig:
            self.cnt[ek] += 1
            c = self.cnt[ek]
            inst.then_inc(self.sems[ek], 1)
        else:
            c = self.cnt[ek] + 1
        for v in reads:
            for g in v.regs:
                g.r[ek] = c
        for v in writes:
            for g in v.regs:
                g.w = (ek, c)
                g.r = {}

    def dma(self, Q, out, in_):
        g = out.regs[0]
        if g.key is None:
            if g.name not in self.keyof:
                self.ndma += 1
                k = "d%d" % self.ndma
                self.keyof[g.name] = k
                self.sems[k] = self.nc.alloc_semaphore(k)
                self.dtot[k] = 0
            g.key = self.keyof[g.name]
        self._need(Q, [in_], [out], skipkey=g.key)
        inst = self.eng[Q].dma_start(out=out.ap, in_=in_.ap)
        self.dtot[g.key] += 16
        c = self.dtot[g.key]
        inst.then_inc(self.sems[g.key], 16)
        for rg in in_.regs:
            rg.r[g.key] = c
        g.w = (g.key, c)
        g.r = {}

    def barrier(self):
        tot = {}
        for e in self.eng:
            ek = self.ekey[e]
            if self.cnt[ek] > 0:
                tot[ek] = self.cnt[ek]
        for E in self.eng:
            sn = self.seen[E]
            for k, c in tot.items():
                if k == self.ekey[E]:
                    continue
                if sn.get(k, 0) < c:
                    self.eng[E].wait_ge(self.sems[k], c)
                    sn[k] = c
            for k, c in self.dtot.items():
                if sn.get(k, 0) < c:
                    self.eng[E].wait_ge(self.sems[k], c)
                    sn[k] = c
        for e in self.eng:
            ek = self.ekey[e]
            if self.cnt[ek] > 12000:
                self.gen[e] += 1
                nk = "%s#%d" % (e, self.gen[e])
                self.ekey[e] = nk
                self.sems[nk] = self.nc.alloc_semaphore("s_%s_%d" % (e, self.gen[e]))
                self.cnt[nk] = 0

    def maybe_barrier(self, limit=24000):
        if any(self.cnt[self.ekey[e]] > limit for e in self.eng):
            self.barrier()

    def mark(self, name):
        self.marks.append((name, self.nmm))

    def mm(self, out, lhsT, rhs, start=True, stop=True, sig=True):
        self.nmm += 1
        self.op("pe", lambda e: e.matmul(out.ap, lhsT.ap, rhs.ap, start=start, stop=stop), [lhsT, rhs], [out], sig=sig)

    def act(self, out, in_, func, scale=1.0, bias=None, E="act"):
        rd = [in_]
        kw = {}
        if bias is not None:
            rd.append(bias)
            kw["bias"] = bias.ap
        if isinstance(scale, V):
            rd.append(scale)
            kw["scale"] = scale.ap
        else:
            kw["scale"] = float(scale)
        self.op("act", lambda e: e.activation(out=out.ap, in_=in_.ap, func=func, **kw), rd, [out])

    def tt(self, out, in0, in1, op, E="dve"):
        self.op(E, lambda e: e.tensor_tensor(out=out.ap, in0=in0.ap, in1=in1.ap, op=op), [in0, in1], [out])

    def ts(self, out, in0, s1, s2, op0, op1=None, E="dve"):
        rd = [in0]
        a1 = s1
        a2 = s2
        if isinstance(s1, V):
            rd.append(s1)
            a1 = s1.ap
        if isinstance(s2, V):
            rd.append(s2)
            a2 = s2.ap
        if op1 is None:
            self.op(E, lambda e: e.tensor_scalar(out=out.ap, in0=in0.ap, scalar1=a1, scalar2=None, op0=op0), rd, [out])
        else:
            self.op(E, lambda e: e.tensor_scalar(out=out.ap, in0=in0.ap, scalar1=a1, scalar2=a2, op0=op0, op1=op1), rd, [out])

    def stt(self, out, in0, sc, in1, op0, op1, E="dve"):
        rd = [in0, in1]
        a = sc
        if isinstance(sc, V):
            rd.append(sc)
            a = sc.ap
        self.op(E, lambda e: e.scalar_tensor_tensor(out=out.ap, in0=in0.ap, scalar=a, in1=in1.ap, op0=op0, op1=op1), rd, [out])

    def recip(self, out, in_):
        self.act(out, in_, AF.Ln)
        self.act(out, out, AF.Exp, scale=-1.0)

    def copy(self, out, in_, E="dve"):
        if E == "act":
            self.act(out, in_, AF.Copy)
        else:
            self.op(E, lambda e: e.tensor_copy(out=out.ap, in_=in_.ap), [in_], [out])

    def memset(self, out, val, E="dve"):
        self.op(E, lambda e: e.memset(out.ap, val), [], [out])


def pipeline(units, A, B, depth=2):
    n = len(units)
    for k in range(min(depth, n)):
        A(units[k])
    for k in range(n):
        if k + depth < n:
            A(units[k + depth])
        B(units[k])


class Arena:
    def __init__(self, kb, start, end):
        self.kb = kb
        self.start = start
        self.end = end
        self.cur = start
        self.hist = []

    def reset(self, to=None):
        self.cur = self.start if to is None else to

    def alloc(self, name, shape, dt, nreg=1):
        n = 1
        for s in shape[1:]:
            n *= s
        nbytes = (n * _dsz(dt) + 63) // 64 * 64
        off = self.cur
        assert off + nbytes <= self.end, "SBUF arena overflow for %s: %d + %d > %d" % (name, off, nbytes, self.end)
        self.cur += nbytes
        b = self.kb.sb(name, shape, dt, off, nreg)
        keep = []
        per = nbytes // nreg
        rng = [(off + i * per, off + (i + 1) * per if i < nreg - 1 else off + nbytes, b.regs[i]) for i in range(nreg)]
        for (s0, e0, r0) in self.hist:
            covered = (off <= s0 and e0 <= off + nbytes)
            for (lo, hi, g) in rng:
                if s0 < hi and lo < e0:
                    if r0.w is not None:
                        k, c = r0.w
                        if g.r.get(k, 0) < c:
                            g.r[k] = c
                    for k, c in r0.r.items():
                        if g.r.get(k, 0) < c:
                            g.r[k] = c
            if not covered:
                keep.append((s0, e0, r0))
        keep.extend(rng)
        self.hist = keep
        return b


def build_program(nseq=NSEQ, nlayer=DEPTH, dbg=None):
    nc = bass.Bass("TRN2", target_bir_lowering=False)
    kb = KB(nc)

    def din(name, shape):
        return nc.dram_tensor(name, list(shape), F32, kind="ExternalInput").ap()

    xT = din("xT", [NSEQ, D, S])
    memT = din("memT", [NSEQ, D, NMEM])
    w_in = din("w_in", [DEPTH, D, D_IN])
    w_out = din("w_out", [DEPTH, D, D])
    xa_wq = din("xa_wq", [DEPTH, D, 512])
    xa_wkv = din("xa_wkv", [DEPTH, D, 1024])
    xa_wo = din("xa_wo", [DEPTH, 512, D])
    w_up = din("ffn_w_up", [DEPTH, D, 2 * D_FF])
    w_down = din("ffn_w_down", [DEPTH, D_FF, D])
    gains = din("gains", [DEPTH, 128, 64])
    gfin = din("gfin", [128, 16])
    colpack = din("colpack", [DEPTH, 128, 146])
    convp = din("convp", [DEPTH, 128, 4, 88])
    w2aug = din("w2aug", [DEPTH, 32, 256])
    cD = din("cD", [128, 2048])
    cmask = din("cmask", [128, 256])
    cU = din("cU", [128, 256])
    augk = din("augk", [8, 4, S])
    augq = din("augq", [8, 4, S])
    yT = nc.dram_tensor("yT", [NSEQ, D, S], F32, kind="ExternalOutput").ap()
    hres = nc.dram_tensor("hres", [NSEQ, D, S], F32, kind="Internal").ap()
    hresB = Buf(None, NSEQ * 2, "hres")
    yTB = Buf(None, NSEQ * 2, "yT")
    inB = Buf(None, 1, "inputs")

    def dv(ap):
        return V(ap, [inB.regs[0]])

    PS = [Buf(nc.alloc_psum_tensor("ps%d" % i, [128, 512], F32), 1, "ps%d" % i) for i in range(8)]
    for b in PS:
        b.regs[0].psum = True
    psS = Rot(PS[0:3])
    psO = Rot(PS[3:5])
    psD = Rot(PS[5:7])
    psX = Rot(PS[7:8])
    psS5 = Rot(PS[0:3] + PS[5:7])
    psO3 = Rot(PS[3:5] + PS[7:8])

    PA = Arena(kb, ARENA0, ARENA0 + 16 * 1024)
    cDt = PA.alloc("cD", [128, 2048], F32)
    cmaskt = PA.alloc("cmask", [128, 256], F32)
    cUt = PA.alloc("cU", [128, 256], F32)
    cUb = PA.alloc("cUb", [128, 128], BF16)
    ones = PA.alloc("ones", [128, 128], BF16)
    gt = PA.alloc("gains", [128, 64], F32)
    gs = PA.alloc("gains_s", [128, 64], F32)
    gfint = PA.alloc("gfin", [128, 16], F32)
    colt = PA.alloc("colpack", [128, 146], F32)
    colx = PA.alloc("colx", [128, 24], F32)
    convt = PA.alloc("convp", [128, 4, 88], F32)
    w2t = PA.alloc("w2aug", [32, 256], BF16)
    carry = PA.alloc("carry", [128, 88, 2], F32)
    epsc = PA.alloc("epsc", [128, 4], F32)
    WA = Arena(kb, PA.end, ARENA_END)

    kb.dma("sp", cDt[:, :], dv(cD[:, :]))
    kb.dma("sp", cmaskt[:, :], dv(cmask[:, :]))
    kb.dma("sp", cUt[:, :], dv(cU[:, :]))
    kb.dma("sp", gfint[:, :], dv(gfin[:, :]))
    kb.memset(ones[:, :], 1.0)
    kb.copy(cUb[:, :], cUt[:, 0:128])
    kb.memset(epsc[:, 0:1], 1.0)
    kb.memset(epsc[:, 1:2], D * EPS)
    kb.memset(epsc[:, 2:3], 64.0 * EPS)
    kb.memset(epsc[:, 3:4], 128.0 * EPS)
    kb.ts(gfint[:, :], gfint[:, :], math.sqrt(D), None, ALU.mult)

    slopes8 = [2.0 ** (-8.0 * (i + 1) / 8) for i in range(8)]
    slopes16 = [2.0 ** (-8.0 * (i + 1) / 16) for i in range(16)]

    def hsrc(l, s):
        return (xT, inB, 0) if l == 0 else (hres, hresB, None)

    def wview(W, l, c0, nc_):
        return dv(W[l, :, c0:c0 + nc_].rearrange("(kc p) n -> p kc n", p=128))

    def rsqrt_to(out, in_, col, M):
        kb.act(out, in_, AF.Ln, bias=epsc[0:M, col:col + 1])
        kb.act(out, out, AF.Exp, scale=-0.5)

    def rms_stats(src_fn, nk, N, sqrot, rs_out, nfeat):
        ps = psX.next()
        for kc in range(nk):
            sq = sqrot.next()
            kb.act(sq[:, 0:N], src_fn(kc), AF.Square)
            kb.mm(ps[:, 0:N], ones[:, :], sq[:, 0:N], start=(kc == 0), stop=(kc == nk - 1))
        rsqrt_to(rs_out, ps[:, 0:N], 1, 128)

    for s in range(nseq):
        for l in range(nlayer):
            lam_init = 0.8 - 0.6 * math.exp(-0.3 * l)
            kb.dma("sp", gt[:, :], dv(gains[l, :, :]))
            kb.dma("sp", colt[:, :], dv(colpack[l, :, :]))
            kb.dma("sp", convt[:, :, :], dv(convp[l, :, :, :]))
            kb.dma("pool", w2t[:, :], dv(w2aug[l, :, :]))
            kb.ts(gs[:, :], gt[:, :], math.sqrt(D), None, ALU.mult)
            kb.ts(colx[:, 0:1], colt[:, 0:1], (1.0 - lam_init) * 8.0, None, ALU.mult)
            kb.ts(colx[:, 1:2], colt[:, 1:2], math.sqrt(128.0), None, ALU.mult)
            kb.act(colx[:, 4:20], colt[:, 2:18], AF.Exp)
            WA.reset()
            lsc = WA.alloc("lsc", [128, 64], F32)
            kb.tt(lsc[:, 0:32], colt[:, 18:50], colt[:, 50:82], ALU.mult)
            kb.tt(lsc[:, 32:64], colt[:, 82:114], colt[:, 114:146], ALU.mult)
            kb.op("dve", lambda e: e.reduce_sum(out=colx[:, 20:21].ap, in_=lsc[:, 0:32].ap, axis=mybir.AxisListType.X), [lsc[:, :]], [colx[:, :]])
            kb.op("dve", lambda e: e.reduce_sum(out=colx[:, 21:22].ap, in_=lsc[:, 32:64].ap, axis=mybir.AxisListType.X), [lsc[:, :]], [colx[:, :]])
            kb.act(colx[:, 22:24], colx[:, 20:22], AF.Exp)
            kb.stt(colx[:, 2:3], colx[:, 23:24], -lam_init, colx[:, 22:23], ALU.add, ALU.subtract)

            src, srcB, srcreg = hsrc(l, s)

            def sreg(half):
                return 0 if srcreg == 0 else s * 2 + half

            WA.reset()
            xn = WA.alloc("xn", [128, 16, S], BF16)
            mixT = WA.alloc("mixT", [128, 16, S], BF16)
            mark = WA.cur
            kb.mark("norm")
            hsts = Rot([WA.alloc("hst%d" % i, [128, 16, 256], F32) for i in range(2)])
            sq2 = [WA.alloc("sq%d" % i, [128, 256], BF16) for i in range(3)]
            rss = Rot([WA.alloc("rs%d" % i, [128, 256], F32) for i in range(2)])
            sqrot = Rot(sq2)
            for t8 in range(S // 256):
                tsl = slice(t8 * 256, (t8 + 1) * 256)
                hst = hsts.next()
                rs = rss.next()
                kb.dma("sp", hst[:, :, :], V(src[s, :, tsl].rearrange("(kc p) t -> p kc t", p=128), [srcB.regs[sreg(t8 // 4)]]))
                rms_stats(lambda kc: hst[:, kc, :], 16, 256, sqrot, rs[:, :], D)
                for kc in range(16):
                    kb.stt(xn[:, kc, tsl], hst[:, kc, :], gs[:, kc:kc + 1], rs[:, :], ALU.mult, ALU.mult)
            WA.reset(mark)
            wb = [WA.alloc("wbm%d" % i, [128, 16, 256], BF16) for i in range(2)]
            wrot = Rot(wb)
            mark2 = WA.cur

            def wload(W, c0, ncols):
                w = wrot.next()
                kb.dma("pool", w[:, :, 0:ncols], wview(W, l, c0, ncols))
                return w

            def proj_fm(w, c0, M, dst_fn, func=AF.Copy, scale=1.0):
                for tl in range(4):
                    ps = psS.next()
                    for kc in range(16):
                        kb.mm(ps[0:M, :], w[:, kc, c0:c0 + M], xn[:, kc, tl * 512:(tl + 1) * 512], start=(kc == 0), stop=(kc == 15), sig=(kc == 15))
                    d = dst_fn(tl)
                    if isinstance(d, list):
                        for (dd, r0, r1) in d:
                            kb.act(dd, ps[r0:r1, :], func, scale=scale)
                    else:
                        kb.act(d, ps[0:M, :], func, scale=scale)

            def proj_tm(w, c0, N, dst_fn, split=None):
                for blk in range(16):
                    ps = psS.next()
                    for kc in range(16):
                        kb.mm(ps[:, 0:N], xn[:, kc, blk * 128:(blk + 1) * 128], w[:, kc, c0:c0 + N], start=(kc == 0), stop=(kc == 15), sig=(kc == 15))
                    if split is None:
                        kb.act(dst_fn(blk), ps[:, 0:N], AF.Copy)
                    else:
                        src_v = V(ps.t[:, 0:N].rearrange("p (h d) -> p h d", h=split[0]), ps.regs)
                        kb.act(dst_fn(blk), src_v, AF.Copy)

            kb.mark("diff")
            sc = 32.0 ** -0.5
            qTh = WA.alloc("qTh", [128, S], BF16)
            kTh = WA.alloc("kTh", [128, S], BF16)
            vdf = WA.alloc("vdf", [128, 16, 4, 128], BF16)
            kb.memset(vdf[:, :, :, 64:128], 1.0)
            Tb = Rot([WA.alloc("T%d" % i, [128, 512], F32) for i in range(2)])
            Pb = Rot([WA.alloc("P%d" % i, [128, 512], BF16) for i in range(4)])
            Rc = WA.alloc("Rc", [64, 512], F32)
            Oc = [WA.alloc("Oc%d" % i, [64, 512], F32) for i in range(2)]
            sqdr = Rot([WA.alloc("sqd%d" % i, [64, 512], BF16) for i in range(1)])
            Oo = Rot([WA.alloc("Oo%d" % i, [64, 512], F32) for i in range(1)])
            rsd = WA.alloc("rsd", [64, 512], F32)
            for hg in range(2):
                wv = wload(w_in, OFF["dv"] + hg * 256, 256)
                proj_tm(wv, 0, 256, lambda blk: vdf[:, blk, :, 0:64], split=(4, 64))
                wq = wload(w_in, OFF["dq"] + hg * 256, 256)
                wk = wload(w_in, OFF["dk"] + hg * 256, 256)
                for hh in range(4):
                    h = hg * 4 + hh
                    if hh == 0:
                        pass
                    proj_fm(wq, hh * 64, 64, lambda tl: [(qTh[0:32, tl * 512:(tl + 1) * 512], 0, 32),
                                                         (qTh[64:96, tl * 512:(tl + 1) * 512], 32, 64)], func=AF.Identity, scale=sc)
                    proj_fm(wk, hh * 64, 64, lambda tl: [(kTh[0:32, tl * 512:(tl + 1) * 512], 0, 32),
                                                         (kTh[64:96, tl * 512:(tl + 1) * 512], 32, 64)])
                    for b0 in (32, 96):
                        kb.dma("pool", qTh[b0:b0 + 4, :], dv(augq[h, :, :]))
                        kb.dma("pool", kTh[b0:b0 + 4, :], dv(augk[h, :, :]))
                    slope = slopes8[h]
                    units = []
                    for i in range(4):
                        for c in range(2):
                            nj = 4 * i + 4
                            for j in range(nj):
                                units.append((i, c, j, nj))
                    Pof = {}
                    acc_of = {}

                    def stA(u, h=h, hh=hh, slope=slope):
                        i, c, j, nj = u
                        qa = max(512 * i, 128 * j)
                        N = 512 * (i + 1) - qa
                        Sp = psS5.next()
                        kb.mm(Sp[:, 0:N], kTh[64 * c:64 * c + 36, 128 * j:128 * j + 128], qTh[64 * c:64 * c + 36, qa:qa + N])
                        P = Pb.next()
                        if qa == 128 * j:
                            T = Tb.next()
                            kb.tt(T[:, 0:128], Sp[:, 0:128], cmaskt[:, 0:128], ALU.add)
                            kb.act(P[:, 0:128], T[:, 0:128], AF.Exp)
                            if N > 128:
                                kb.act(P[:, 128:N], Sp[:, 128:N], AF.Exp)
                        else:
                            kb.act(P[:, 0:N], Sp[:, 0:N], AF.Exp)
                        Pof[u] = P

                    def stB(u, h=h, hh=hh):
                        i, c, j, nj = u
                        qa = max(512 * i, 128 * j)
                        N = 512 * (i + 1) - qa
                        off = qa - 512 * i
                        if j == 0:
                            acc_of[(i, c)] = psO.next()
                        O = acc_of[(i, c)]
                        P = Pof.pop(u)
                        kb.mm(O[:, off:off + N], vdf[:, j, hh, :], P[:, 0:N], start=(j == 0), stop=(j == nj - 1))
                        if j == nj - 1:
                            kb.recip(Rc[:, :], O[64:128, :])
                            kb.tt(Oc[c][:, :], O[0:64, :], Rc[:, :], ALU.mult)
                            if c == 1:
                                oo = Oo.next()
                                sqq = sqdr.next()
                                kb.stt(oo[:, :], Oc[1][:, :], colx[0:64, 2:3], Oc[0][:, :], ALU.mult, ALU.add)
                                kb.act(sqq[:, :], oo[:, :], AF.Square)

                                def tail(i=i, oo=oo, sqq=sqq, h=h):
                                    pn = psX.next()
                                    kb.mm(pn[0:64, :], ones[0:64, 0:64], sqq[:, :])
                                    rsqrt_to(rsd[:, :], pn[0:64, :], 2, 64)
                                    po = (h % 2) * 64
                                    kb.stt(mixT[po:po + 64, h // 2, i * 512:(i + 1) * 512], oo[:, :], colx[0:64, 0:1], rsd[:, :], ALU.mult, ALU.mult)

                                deferred.append([3, tail])
                        for dt_ in deferred:
                            dt_[0] -= 1
                        while deferred and deferred[0][0] <= 0:
                            deferred.pop(0)[1]()

                    deferred = []
                    pipeline(units, stA, stB, 3)
                    while deferred:
                        deferred.pop(0)[1]()
            if dbg == "diff":
                return finish_dbg(nc, kb, mixT, yT, yTB, s)
            WA.reset(mark2)
            kb.mark("swa")
            sc = 64.0 ** -0.5
            qTs = WA.alloc("qTs", [128, 4, S], BF16)
            kTs = WA.alloc("kTs", [128, S], BF16)
            vsw = WA.alloc("vsw", [128, 16, 2, 128], BF16)
            kb.memset(vsw[:, :, :, 64:128], 1.0)
            Tb = Rot([WA.alloc("T%d" % i, [128, 512], F32) for i in range(2)])
            Pb = Rot([WA.alloc("P%d" % i, [128, 512], BF16) for i in range(4)])
            Rs = WA.alloc("Rs", [64, 512], F32)
            bdiag = WA.alloc("bdiag", [128, 512], F32)
            bprev = WA.alloc("bprev", [128, 512], F32)
            wkv = wload(w_in, OFF["sk"], 256)
            proj_fm(wkv, 0, 128, lambda tl: kTs[:, tl * 512:(tl + 1) * 512])
            proj_tm(wkv, 128, 128, lambda blk: vsw[:, blk, :, 0:64], split=(2, 64))
            for rh in range(2):
                for g in range(2):
                    w = wload(w_in, OFF["sq"] + (8 * g + 4 * rh) * 64, 256)
                    for pr in range(2):
                        rrA = 2 * pr
                        proj_fm(w, pr * 128, 128, lambda tl, rrA=rrA, g=g: [
                            (qTs[g * 64:(g + 1) * 64, rrA, tl * 512:(tl + 1) * 512], 0, 64),
                            (qTs[g * 64:(g + 1) * 64, rrA + 1, tl * 512:(tl + 1) * 512], 64, 128)])
                for g in range(2):
                    for rr in range(4):
                        slope = slopes16[8 * g + 4 * rh + rr]
                        seg = slice(rr * 128, (rr + 1) * 128)
                        kb.stt(bdiag[:, seg], cDt[:, 0:128], -slope / sc, cmaskt[:, 0:128], ALU.mult, ALU.add)
                        kb.stt(bprev[:, seg], cDt[:, 128:256], -slope / sc, cmaskt[:, 128:256], ALU.mult, ALU.add)
                    units = []
                    for n in range(16):
                        ms = [n - 1, n] if n > 0 else [n]
                        for idx, m in enumerate(ms):
                            units.append((n, m, idx, len(ms)))
                    Pof = {}
                    acc_of = {}

                    def stA(u, g=g):
                        n, m, idx, nm = u
                        Sp = psS5.next()
                        kb.mm(Sp[:, :], kTs[g * 64:(g + 1) * 64, m * 128:(m + 1) * 128], qTs[g * 64:(g + 1) * 64, :, n * 128:(n + 1) * 128])
                        T = Tb.next()
                        kb.tt(T[:, :], Sp[:, :], (bdiag if m == n else bprev)[:, :], ALU.add)
                        P = Pb.next()
                        kb.act(P[:, :], T[:, :], AF.Exp, scale=sc)
                        Pof[u] = P

                    def stB(u, g=g, rh=rh):
                        n, m, idx, nm = u
                        if idx == 0:
                            acc_of[n] = psO3.next()
                        O = acc_of[n]
                        P = Pof.pop(u)
                        kb.mm(O[:, :], vsw[:, m, g, :], P[:, :], start=(idx == 0), stop=(idx == nm - 1))
                        if idx == nm - 1:
                            for rr in range(4):
                                hd = 8 * g + 4 * rh + rr
                                kb.ts(Rs[:, rr * 128:(rr + 1) * 128], O[64:128, rr * 128:(rr + 1) * 128], colx[64:128, 4 + hd:5 + hd], None, ALU.add)
                            kb.recip(Rs[:, :], Rs[:, :])
                            for rr in range(4):
                                hd = 8 * g + 4 * rh + rr
                                po = (hd % 2) * 64
                                kb.tt(mixT[po:po + 64, 4 + hd // 2, n * 128:(n + 1) * 128], O[0:64, rr * 128:(rr + 1) * 128], Rs[:, rr * 128:(rr + 1) * 128], ALU.mult)

                    pipeline(units, stA, stB, 3)
            if dbg == "swa":
                return finish_dbg(nc, kb, mixT, yT, yTB, s)
            WA.reset(mark2)
            kb.mark("gla")
            glr = WA.alloc("glr", [32, S], BF16)
            qTg = WA.alloc("qTg", [64, S], BF16)
            kTg = WA.alloc("kTg", [64, S], BF16)
            ktk = WA.alloc("ktk", [128, 16, 64], BF16)
            vtk = WA.alloc("vtk", [128, 16, 128], BF16)
            sgo = WA.alloc("sgo", [128, S], BF16)
            lap = Rot([WA.alloc("lap%d" % i, [128, 64], F32) for i in range(2)])
            e1b = Rot([WA.alloc("e1%d" % i, [128, 64], F32) for i in range(2)])
            eeb = Rot([WA.alloc("ee%d" % i, [128, 64], F32) for i in range(2)])
            kend = Rot([WA.alloc("kend%d" % i, [128, 64], BF16) for i in range(4)])
            ebb = Rot([WA.alloc("eb%d" % i, [64, 128], F32) for i in range(4)])
            ebi = Rot([WA.alloc("ebi%d" % i, [64, 128], F32) for i in range(2)])
            qdec = Rot([WA.alloc("qdec%d" % i, [64, 128], BF16) for i in range(4)])
            kinv = Rot([WA.alloc("kinv%d" % i, [64, 128], BF16) for i in range(2)])
            attn = Rot([WA.alloc("attn%d" % i, [128, 128], BF16) for i in range(4)])
            Sf = Rot([WA.alloc("Sf%d" % i, [64, 128], F32) for i in range(3)])
            Sb = Rot([WA.alloc("Sb%d" % i, [64, 128], BF16) for i in range(3)])
            sqg = Rot([WA.alloc("sqg%d" % i, [128, 128], BF16) for i in range(2)])
            rsg = Rot([WA.alloc("rsg%d" % i, [128, 128], F32) for i in range(2)])
            t1g = Rot([WA.alloc("t1g%d" % i, [128, 128], F32) for i in range(2)])
            kb.memset(glr[:, :], 1.0)
            wl = wload(w_in, OFF["glr"], 16)
            proj_fm(wl, 0, 16, lambda tl: glr[0:16, tl * 512:(tl + 1) * 512])
            for hh in range(4):
                w = wload(w_in, OFF["gq"] + hh * 64, 64)
                proj_fm(w, 0, 64, lambda tl: qTg[:, tl * 512:(tl + 1) * 512])
                w = wload(w_in, OFF["gk"] + hh * 64, 64)
                proj_fm(w, 0, 64, lambda tl: kTg[:, tl * 512:(tl + 1) * 512])
                proj_tm(w, 0, 64, lambda blk: ktk[:, blk, :])
                w = wload(w_in, OFF["gv"] + hh * 128, 128)
                proj_tm(w, 0, 128, lambda blk: vtk[:, blk, :])
                w = wload(w_in, OFF["gog"] + hh * 128, 128)
                proj_fm(w, 0, 128, lambda tl: sgo[:, tl * 512:(tl + 1) * 512], func=AF.Silu)
                st = {}
                st["Sfc"] = Sf.next()
                st["Sbc"] = Sb.next()
                kb.memset(st["Sfc"][:, :], 0.0)
                kb.memset(st["Sbc"][:, :], 0.0)
                blk = {}

                def gA(nb, hh=hh):
                    bs = slice(nb * 128, (nb + 1) * 128)
                    z = psS.next()
                    kb.mm(z[:, 0:64], glr[0:17, bs], w2t[0:17, hh * 64:(hh + 1) * 64])
                    e1 = e1b.next()
                    kb.act(e1[:, :], z[:, 0:64], AF.Exp, scale=-1.0)
                    lp = lap.next()
                    kb.act(lp[:, :], e1[:, :], AF.Ln, bias=epsc[:, 0:1])
                    et = psS.next()
                    kb.mm(et[:, 0:64], cUt[:, 128:256], lp[:, :])
                    bT = psS.next()
                    kb.mm(bT[0:64, 0:128], lp[:, :], cUt[:, 0:128])
                    ee = eeb.next()
                    kb.act(ee[:, :], et[:, 0:64], AF.Exp, scale=-1.0 / 16.0)
                    ke = kend.next()
                    kb.tt(ke[:, :], ktk[:, nb, :], ee[:, :], ALU.mult)
                    eb = ebb.next()
                    kb.act(eb[:, :], bT[0:64, 0:128], AF.Exp, scale=-1.0 / 16.0)
                    ei = ebi.next()
                    kb.act(ei[:, :], bT[0:64, 0:128], AF.Exp, scale=1.0 / 16.0)
                    qd = qdec.next()
                    kb.stt(qd[:, :], qTg[:, bs], 0.125, eb[:, :], ALU.mult, ALU.mult)
                    ki = kinv.next()
                    kb.tt(ki[:, :], kTg[:, bs], ei[:, :], ALU.mult)
                    at = psS.next()
                    kb.mm(at[:, 0:128], ki[:, :], qd[:, :])
                    an = attn.next()
                    kb.tt(an[:, :], at[:, 0:128], cUt[:, 0:128], ALU.mult)
                    blk[nb] = (ke, eb, qd, an)

                kvof = {}

                def gK(nb, hh=hh):
                    ke = blk[nb][0]
                    kv0 = psD.next()
                    kb.mm(kv0[0:64, 0:128], ke[0:64, :], vtk[0:64, nb, :])
                    kv1 = psD.next()
                    kb.mm(kv1[0:64, 0:128], ke[64:128, :], vtk[64:128, nb, :])
                    kvof[nb] = (kv0, kv1)

                def gB(nb, hh=hh):
                    bs = slice(nb * 128, (nb + 1) * 128)
                    ke, eb, qd, an = blk.pop(nb)
                    kv0, kv1 = kvof.pop(nb)
                    Sfc = st["Sfc"]
                    Sbc = st["Sbc"]
                    Sfn = Sf.next()
                    kb.stt(Sfn[:, :], Sfc[:, :], eb[:, 63:64], kv0[0:64, 0:128], ALU.mult, ALU.add)
                    Sf2 = Sf.next()
                    kb.stt(Sf2[:, :], Sfn[:, :], eb[:, 127:128], kv1[0:64, 0:128], ALU.mult, ALU.add)
                    if nb + 1 < 16:
                        gK(nb + 1)
                    Sbn = Sb.next()
                    kb.copy(Sbn[:, :], Sfn[:, :], E="act")
                    Sb2 = Sb.next()
                    kb.copy(Sb2[:, :], Sf2[:, :], E="act")
                    o = psO.next()
                    kb.mm(o[:, 0:128], vtk[:, nb, :], an[:, :], start=True, stop=False)
                    kb.mm(o[:, 0:64], Sbc[:, :], qd[:, 0:64], start=False, stop=False)
                    kb.mm(o[:, 64:128], Sbn[:, :], qd[:, 64:128], start=False, stop=True)
                    st["Sfc"] = Sf2
                    st["Sbc"] = Sb2
                    sq = sqg.next()
                    kb.act(sq[:, :], o[:, 0:128], AF.Square)
                    pn = psX.next()
                    kb.mm(pn[:, 0:128], ones[:, :], sq[:, :])
                    rg_ = rsg.next()
                    rsqrt_to(rg_[:, :], pn[:, 0:128], 3, 128)
                    t1 = t1g.next()
                    kb.stt(t1[:, :], o[:, 0:128], colx[:, 1:2], rg_[:, :], ALU.mult, ALU.mult)
                    kb.tt(mixT[:, 12 + hh, bs], t1[:, :], sgo[:, bs], ALU.mult)

                gA(0)
                gA(1)
                gK(0)
                for nb in range(16):
                    if nb + 2 < 16:
                        gA(nb + 2)
                    gB(nb)
            if dbg == "gla":
                return finish_dbg(nc, kb, mixT, yT, yTB, s)
            WA.reset(mark2)
            kb.mark("wout")
            hold = Rot([WA.alloc("hold%d" % i, [128, 512], F32) for i in range(4)])
            for cg in range(8):
                w = wload(w_out, cg * 256, 256)
                for dc in range(2):
                    dch = cg * 2 + dc
                    for tl in range(4):
                        tsl = slice(tl * 512, (tl + 1) * 512)
                        ho = hold.next()
                        kb.dma("sp", ho[:, :], V(src[s, dch * 128:(dch + 1) * 128, tsl], [srcB.regs[sreg(tl // 2)]]))
                        ps = psS.next()
                        for kc in range(16):
                            kb.mm(ps[:, :], w[:, kc, dc * 128:(dc + 1) * 128], mixT[:, kc, tsl], start=(kc == 0), stop=(kc == 15), sig=(kc == 15))
                        kb.tt(ho[:, :], ho[:, :], ps[:, :], ALU.add)
                        kb.dma("sp", V(hres[s, dch * 128:(dch + 1) * 128, tsl], [hresB.regs[s * 2 + tl // 2]]), ho[:, :])
            if dbg == "mix":
                return finish_dbg2(nc, kb, hres, hresB, yT, yTB, s)

            WA.reset()
            kTx = WA.alloc("kTx", [128, 4, 256], BF16)
            vx = WA.alloc("vx", [128, 2, 512], BF16)
            markKV = WA.cur
            kb.mark("xkv")
            mst = WA.alloc("mst", [128, 16, 128], F32)
            memn = WA.alloc("memn", [128, 16, 256], BF16)
            wbk = Rot([WA.alloc("wbk%d" % i, [128, 16, 256], BF16) for i in range(2)])
            sqk = Rot([WA.alloc("sqk%d" % i, [128, 128], BF16) for i in range(2)])
            rsk = WA.alloc("rsk", [128, 128], F32)
            for mb in range(2):
                kb.dma("sp", mst[:, :, :], dv(memT[s, :, mb * 128:(mb + 1) * 128].rearrange("(kc p) t -> p kc t", p=128)))
                rms_stats(lambda kc: mst[:, kc, :], 16, 128, sqk, rsk[:, :], D)
                for kc in range(16):
                    kb.stt(memn[:, kc, mb * 128:(mb + 1) * 128], mst[:, kc, :], gs[:, 32 + kc:33 + kc], rsk[:, :], ALU.mult, ALU.mult)
            for cgk in range(2):
                w = wbk.next()
                kb.dma("pool", w[:, :, :], wview(xa_wkv, l, cgk * 256, 256))
                for hx2 in range(2):
                    hx = cgk * 2 + hx2
                    ps = psS.next()
                    for kc in range(16):
                        kb.mm(ps[:, 0:256], w[:, kc, hx2 * 128:(hx2 + 1) * 128], memn[:, kc, :], start=(kc == 0), stop=(kc == 15), sig=(kc == 15))
                    kb.act(kTx[:, hx, :], ps[:, 0:256], AF.Copy)
            for cgv in range(2):
                w = wbk.next()
                kb.dma("pool", w[:, :, :], wview(xa_wkv, l, 512 + cgv * 256, 256))
                for mb in range(2):
                    ps = psS.next()
                    for kc in range(16):
                        kb.mm(ps[:, 0:256], memn[:, kc, mb * 128:(mb + 1) * 128], w[:, kc, :], start=(kc == 0), stop=(kc == 15), sig=(kc == 15))
                    kb.act(vx[:, mb, cgv * 256:(cgv + 1) * 256], ps[:, 0:256], AF.Copy)
            for hf in range(2):
                WA.reset(markKV)
                T0 = hf * 1024
                acc = WA.alloc("acc", [128, 16, 1024], F32, nreg=16)

                def A(kc, sl):
                    return acc.rg(kc, (slice(None), kc, sl))

                xn2 = WA.alloc("xn2", [128, 16, 1024], BF16)
                wb = [WA.alloc("wb%d" % i, [128, 16, 256], BF16) for i in range(3)]
                wrot = Rot(wb)
                sq2 = [WA.alloc("sq%d" % i, [128, 512], BF16) for i in range(2)]
                sqrot = Rot(sq2)
                rs5 = WA.alloc("rs5", [128, 512], F32)
                markT = WA.cur
                for kc in range(16):
                    kb.dma("sp", A(kc, slice(None)), V(hres[s, kc * 128:(kc + 1) * 128, T0:T0 + 1024], [hresB.regs[s * 2 + hf]]))

                def norm_to_xn2(gcol0):
                    for tl in range(2):
                        tsl = slice(tl * 512, (tl + 1) * 512)
                        rms_stats(lambda kc: A(kc, tsl), 16, 512, sqrot, rs5[:, :], D)
                        for kc in range(16):
                            kb.stt(xn2[:, kc, tsl], A(kc, tsl), gs[:, gcol0 + kc:gcol0 + kc + 1], rs5[:, :], ALU.mult, ALU.mult)

                kb.mark("xa%d" % hf)
                qTx = WA.alloc("qTx", [128, 4, 1024], BF16)
                oxa = WA.alloc("oxa", [128, 4, 1024], BF16)
                wo = WA.alloc("wo", [128, 4, 2048], BF16)
                Rx = WA.alloc("Rx", [128, 512], F32)
                Pb = Rot([WA.alloc("P%d" % i, [128, 512], BF16) for i in range(3)])
                norm_to_xn2(16)
                for cgq in range(2):
                    w = wrot.next()
                    kb.dma("pool", w[:, :, :], wview(xa_wq, l, cgq * 256, 256))
                    for hx2 in range(2):
                        hx = cgq * 2 + hx2
                        for tl in range(2):
                            ps = psS.next()
                            for kc in range(16):
                                kb.mm(ps[:, :], w[:, kc, hx2 * 128:(hx2 + 1) * 128], xn2[:, kc, tl * 512:(tl + 1) * 512], start=(kc == 0), stop=(kc == 15), sig=(kc == 15))
                            kb.act(qTx[:, hx, tl * 512:(tl + 1) * 512], ps[:, :], AF.Copy)
                kb.dma("pool", wo[:, :, :], dv(xa_wo[l, :, :].rearrange("(kc p) n -> p kc n", p=128)))
                scx = 128.0 ** -0.5
                units = []
                for hx in range(4):
                    for tl in range(2):
                        for mb in range(2):
                            units.append((hx, tl, mb))
                Pof = {}
                acc_of = {}

                def xA(u):
                    hx, tl, mb = u
                    tsl = slice(tl * 512, (tl + 1) * 512)
                    Sp = psS.next()
                    kb.mm(Sp[:, :], kTx[:, hx, mb * 128:(mb + 1) * 128], qTx[:, hx, tsl])
                    P = Pb.next()
                    kb.act(P[:, :], Sp[:, :], AF.Exp, scale=scx)
                    Pof[u] = P

                def xB(u):
                    hx, tl, mb = u
                    tsl = slice(tl * 512, (tl + 1) * 512)
                    if mb == 0:
                        acc_of[(hx, tl)] = (psO.next(), psD.next())
                    O, Dn = acc_of[(hx, tl)]
                    P = Pof.pop(u)
                    kb.mm(O[:, :], vx[:, mb, hx * 128:(hx + 1) * 128], P[:, :], start=(mb == 0), stop=(mb == 1))
                    kb.mm(Dn[:, :], ones[:, :], P[:, :], start=(mb == 0), stop=(mb == 1))
                    if mb == 1:
                        kb.recip(Rx[:, :], Dn[:, :])
                        kb.tt(oxa[:, hx, tsl], O[:, :], Rx[:, :], ALU.mult)

                pipeline(units, xA, xB, 2)
                for dch in range(16):
                    for tl in range(2):
                        tsl = slice(tl * 512, (tl + 1) * 512)
                        ps = psS.next()
                        for kc in range(4):
                            kb.mm(ps[:, :], wo[:, kc, dch * 128:(dch + 1) * 128], oxa[:, kc, tsl], start=(kc == 0), stop=(kc == 3), sig=(kc == 3))
                        kb.tt(A(dch, tsl), A(dch, tsl), ps[:, :], ALU.add)
                if dbg == "xa":
                    return finish_dbg3(nc, kb, acc, yT, yTB, s, hf)
                WA.reset(markT)
                kb.mark("ffn%d" % hf)
                norm_to_xn2(48)
                G = 4
                wdn = Rot([WA.alloc("wd%d" % i, [128, 2048], BF16) for i in range(7)])
                actb = Rot([WA.alloc("act%d" % i, [128, 1024], BF16) for i in range(7)])
                ag = Rot([WA.alloc("ag%d" % i, [128, 512], F32) for i in range(2)])
                au = Rot([WA.alloc("au%d" % i, [128, 512], F32) for i in range(2)])
                sgb = Rot([WA.alloc("sg%d" % i, [128, 512], F32) for i in range(2)])
                psG = Rot(PS[0:4])
                psDn = Rot(PS[4:7])
                npair = D_FF // 128
                def ffn_down(grp):
                    for dch in range(16):
                        for tl in range(2):
                            tsl = slice(tl * 512, (tl + 1) * 512)
                            ps = psDn.next()
                            for ki, (wd, ab) in enumerate(grp):
                                kb.mm(ps[:, :], wd[:, dch * 128:(dch + 1) * 128], ab[:, tsl], start=(ki == 0), stop=(ki == len(grp) - 1), sig=(ki == len(grp) - 1))
                            kb.tt(A(dch, tsl), A(dch, tsl), ps[:, :], ALU.add)

                pending = None
                grp = []
                for p in range(npair):
                    wu = wrot.next()
                    kb.dma("pool", wu[:, :, 0:128], wview(w_up, l, p * 128, 128))
                    kb.dma("pool", wu[:, :, 128:256], wview(w_up, l, D_FF + p * 128, 128))
                    wd = wdn.next()
                    kb.dma("pool", wd[:, :], dv(w_down[l, p * 128:(p + 1) * 128, :]))
                    ab = actb.next()
                    grp.append((wd, ab))
                    for tl in range(2):
                        tsl = slice(tl * 512, (tl + 1) * 512)
                        pg = psG.next()
                        pu = psG.next()
                        for kc in range(16):
                            kb.mm(pg[:, :], wu[:, kc, 0:128], xn2[:, kc, tsl], start=(kc == 0), stop=(kc == 15), sig=(kc == 15))
                        for kc in range(16):
                            kb.mm(pu[:, :], wu[:, kc, 128:256], xn2[:, kc, tsl], start=(kc == 0), stop=(kc == 15), sig=(kc == 15))
                        first = (hf == 0 and tl == 0)
                        res = []
                        for (pp, ch, abuf) in ((pg, p, ag), (pu, npair + p, au)):
                            a = abuf.next()
                            kb.act(a[:, :], pp[:, :], AF.Identity, scale=convt[:, 2, ch:ch + 1], bias=convt[:, 3, ch:ch + 1])
                            kb.stt(a[:, 1:512], pp[:, 0:511], convt[:, 1, ch:ch + 1], a[:, 1:512], ALU.mult, ALU.add)
                            kb.stt(a[:, 2:512], pp[:, 0:510], convt[:, 0, ch:ch + 1], a[:, 2:512], ALU.mult, ALU.add)
                            if not first:
                                kb.stt(a[:, 0:1], carry[:, ch, 1:2], convt[:, 1, ch:ch + 1], a[:, 0:1], ALU.mult, ALU.add)
                                kb.stt(a[:, 0:2], carry[:, ch, 0:2], convt[:, 0, ch:ch + 1], a[:, 0:2], ALU.mult, ALU.add)
                            kb.copy(carry[:, ch, :], pp[:, 510:512], E="act")
                            res.append(a)
                        sg = sgb.next()
                        kb.act(sg[:, :], res[0][:, :], AF.Silu)
                        kb.tt(ab[:, tsl], sg[:, :], res[1][:, :], ALU.mult)
                    if pending is not None:
                        ffn_down(pending)
                        pending = None
                    if len(grp) == G or p == npair - 1:
                        pending = grp
                        grp = []
                if pending is not None:
                    ffn_down(pending)
                if l == nlayer - 1:
                    for tl in range(2):
                        tsl = slice(tl * 512, (tl + 1) * 512)
                        rms_stats(lambda kc: A(kc, tsl), 16, 512, sqrot, rs5[:, :], D)
                        for kc in range(16):
                            kb.stt(A(kc, tsl), A(kc, tsl), gfint[:, kc:kc + 1], rs5[:, :], ALU.mult, ALU.mult)
                    for kc in range(16):
                        kb.dma("sp", V(yT[s, kc * 128:(kc + 1) * 128, T0:T0 + 1024], [yTB.regs[s * 2 + hf]]), A(kc, slice(None)))
                else:
                    for kc in range(16):
                        kb.dma("sp", V(hres[s, kc * 128:(kc + 1) * 128, T0:T0 + 1024], [hresB.regs[s * 2 + hf]]), A(kc, slice(None)))
            kb.maybe_barrier()
    kb.mark("end")
    finish(nc, kb)
    global LAST_KB
    LAST_KB = kb
    return nc


def _collect_dtot(kb, bufs):
    pass


def finish(nc, kb):
    kb.barrier()


def finish_dbg(nc, kb, mixT, yT, yTB, s):
    for kc in range(16):
        kb.dma("pool", V(yT[s, kc * 128:(kc + 1) * 128, :], [yTB.regs[0]]), mixT[:, kc, :])
    kb.barrier()
    return nc


def finish_dbg2(nc, kb, hres, hresB, yT, yTB, s):
    kb.dma("sp", V(yT[s, :, :], [yTB.regs[0]]), V(hres[s, :, :], [hresB.regs[2 * s], hresB.regs[2 * s + 1]]))
    kb.barrier()
    return nc


def finish_dbg3(nc, kb, acc, yT, yTB, s, hf):
    for kc in range(16):
        kb.dma("sp", V(yT[s, kc * 128:(kc + 1) * 128, hf * 1024:(hf + 1) * 1024], [yTB.regs[0]]), V(acc.t[:, kc, :], acc.regs))
    kb.barrier()
    return nc


def host_consts():
    p = np.arange(128, dtype=np.float32)[:, None]
    u = np.arange(2048, dtype=np.float32)[None, :]
    cD = (u - p).astype(np.float32)
    c = np.arange(128)[None, :]
    pp = np.arange(128)[:, None]
    diag = np.where(c >= pp, 0.0, NEG).astype(np.float32)
    prev = np.where(c < pp, 0.0, NEG).astype(np.float32)
    cmask = np.concatenate([diag, prev], axis=1)
    same = (pp // 64) == (c // 64)
    uincl = (same & (pp <= c)).astype(np.float32)
    uafter = (same & (pp > c)).astype(np.float32)
    cU = np.concatenate([uincl, uafter], axis=1)
    return cD, cmask, cU


def prep_shared(inp):
    f = np.float32
    L = DEPTH

    def col16(g):
        return np.ascontiguousarray(g.reshape(L, 16, 128).transpose(0, 2, 1))

    gains = np.concatenate([col16(inp["norm_mix_g"]), col16(inp["norm_xa_g"]), col16(inp["norm_mem_g"]), col16(inp["norm_ffn_g"])], axis=2).astype(f)
    gfin = np.ascontiguousarray(inp["final_norm_g"].reshape(16, 128).T).astype(f)
    colpack = np.zeros((L, 128, 146), f)
    colpack[:, :, 0] = np.concatenate([inp["diff_subln_g"], inp["diff_subln_g"]], axis=1)
    colpack[:, :, 1] = inp["gla_norm_g"]
    colpack[:, :, 2:18] = inp["swa_sinks"][:, None, :]
    colpack[:, :, 18:146] = inp["diff_lambda"].reshape(L, 1, 128)
    cw = inp["ffn_conv_w"].reshape(L, 3, 88, 128)
    cb = inp["ffn_conv_b"].reshape(L, 1, 88, 128)
    convp = np.ascontiguousarray(np.concatenate([cw, cb], axis=1).transpose(0, 3, 1, 2)).astype(f)
    w2aug = np.zeros((L, 32, 256), f)
    w2aug[:, 0:16, :] = inp["gla_gate_w2"]
    w2aug[:, 16, :] = inp["gla_gate_b"]
    cD, cmask, cU = host_consts()
    tpos = np.arange(S)
    ka = (tpos // 128).astype(f)
    kbb = (tpos % 128).astype(f)
    augk = np.zeros((8, 4, S), f)
    augq = np.zeros((8, 4, S), f)
    for hh_ in range(8):
        sl = 2.0 ** (-(hh_ + 1))
        augk[hh_, 0] = ka
        augk[hh_, 1] = kbb
        augk[hh_, 2] = sl
        augk[hh_, 3] = sl
        augq[hh_, 0] = 128.0 * sl
        augq[hh_, 1] = sl
        augq[hh_, 2] = -128.0 * ka
        augq[hh_, 3] = -kbb
    sh = dict(w_in=inp["w_in"], w_out=inp["w_out"], xa_wq=inp["xa_wq"], xa_wkv=inp["xa_wkv"], xa_wo=inp["xa_wo"],
              ffn_w_up=inp["ffn_w_up"], ffn_w_down=inp["ffn_w_down"], gains=gains, gfin=gfin, colpack=colpack,
              convp=convp, w2aug=w2aug, cD=cD, cmask=cmask, cU=cU, augk=augk, augq=augq)
    return {k: np.ascontiguousarray(v, dtype=f) for k, v in sh.items()}


def kernel(**inputs):
    inp = {k: np.asarray(v) for k, v in inputs.items()}
    ncores = 8
    shared = prep_shared(inp)
    x = inp["x"]
    mem = inp["mem"]
    in_maps = []
    for c in range(ncores):
        m = dict(shared)
        m["xT"] = np.ascontiguousarray(x[c * NSEQ:(c + 1) * NSEQ].transpose(0, 2, 1), dtype=np.float32)
        m["memT"] = np.ascontiguousarray(mem[c * NSEQ:(c + 1) * NSEQ].transpose(0, 2, 1), dtype=np.float32)
        in_maps.append(m)
    nc = build_program()
    res = run_bass_kernel_spmd(nc, in_maps, core_ids=list(range(ncores)))
    out = np.empty((16, S, D), np.float32)
    for c in range(ncores):
        yT = res.results[c]["yT"]
        out[c * NSEQ:(c + 1) * NSEQ] = yT.transpose(0, 2, 1)
    return out
```
